# Optimizing a Trainium2 kernel written in Bass

```python
import jax
import jax.numpy as jnp
from jax import lax
import numpy as np

D_MODEL = 1024
BATCH = 8
SEQ = 2048
DEPTH = 4

ATT_HEADS = 8
ATT_KV_HEADS = 2
ATT_HEAD_DIM = 64
ATT_GROUP = ATT_HEADS // ATT_KV_HEADS
ATT_WIDTH = ATT_HEADS * ATT_HEAD_DIM
KV_WIDTH = ATT_KV_HEADS * ATT_HEAD_DIM
WINDOW = 128
ATT_BLOCK = 128
ROPE_THETA = 500000.0
ROPE_DIM = ATT_HEAD_DIM // 4

HG_HEADS = 4
HG_HEAD_DIM = 128
HG_WIDTH = HG_HEADS * HG_HEAD_DIM
HG_CHUNK = 64

MIX_WIDTH = ATT_WIDTH + HG_WIDTH
IN_WIDTH = ATT_WIDTH + 2 * KV_WIDTH + 5 * HG_WIDTH

N_EXPERTS = 32
TOP_K = 4
D_FF = 1024
SWIGLU_ALPHA = 1.702
SWIGLU_LIMIT = 7.0
MOE_BLOCK = 256

N_MOD = 6
EPS = 1e-6
NEG_INF = -1e30
LB_FLOOR = 1e-30

kernel_name = "hymba_swa_hgrn2_moe_adaln_encoder"


def rms_norm(x, gain):
    xf = x.astype(jnp.float32)
    y = xf * lax.rsqrt(jnp.mean(xf * xf, axis=-1, keepdims=True) + EPS)
    return (y * gain.astype(jnp.float32)).astype(x.dtype)


def partial_rope(x, positions):
    half = ROPE_DIM // 2
    inv_freq = ROPE_THETA ** (-(jnp.arange(half, dtype=jnp.float32) * 2.0 / ROPE_DIM))
    ang = positions.astype(jnp.float32)[..., None] * inv_freq
    cos = jnp.cos(ang)[:, :, None, :]
    sin = jnp.sin(ang)[:, :, None, :]
    xr = x[..., :ROPE_DIM].astype(jnp.float32)
    x1, x2 = xr[..., :half], xr[..., half:]
    rot = jnp.concatenate([x1 * cos - x2 * sin, x2 * cos + x1 * sin], axis=-1).astype(x.dtype)
    return jnp.concatenate([rot, x[..., ROPE_DIM:]], axis=-1)


def window_attention(q, k, v, sink):
    B, S = q.shape[0], q.shape[1]
    nb = S // ATT_BLOCK
    qb = q.reshape(B, nb, ATT_BLOCK, ATT_KV_HEADS, ATT_GROUP, ATT_HEAD_DIM)
    pad = ((0, 0), (ATT_BLOCK, ATT_BLOCK), (0, 0), (0, 0))
    kp = jnp.pad(k, pad).reshape(B, nb + 2, ATT_BLOCK, ATT_KV_HEADS, ATT_HEAD_DIM)
    vp = jnp.pad(v, pad).reshape(B, nb + 2, ATT_BLOCK, ATT_KV_HEADS, ATT_HEAD_DIM)
    kb = jnp.concatenate([kp[:, :-2], kp[:, 1:-1], kp[:, 2:]], axis=2)
    vb = jnp.concatenate([vp[:, :-2], vp[:, 1:-1], vp[:, 2:]], axis=2)
    s = jnp.einsum("bnqhgd,bnshd->bnhgqs", qb, kb,
                   preferred_element_type=jnp.float32) * (ATT_HEAD_DIM ** -0.5)
    blk = jnp.arange(nb)[:, None, None]
    qpos = blk * ATT_BLOCK + jnp.arange(ATT_BLOCK)[None, :, None]
    kpos = (blk - 1) * ATT_BLOCK + jnp.arange(3 * ATT_BLOCK)[None, None, :]
    valid = (jnp.abs(qpos - kpos) <= WINDOW) & (kpos >= 0) & (kpos < S)
    s = jnp.where(valid[None, :, None, None], s, NEG_INF)
    sink_col = jnp.broadcast_to(
        sink.astype(jnp.float32).reshape(1, 1, ATT_KV_HEADS, ATT_GROUP, 1, 1), s.shape[:-1] + (1,))
    p = jax.nn.softmax(jnp.concatenate([s, sink_col], axis=-1), axis=-1)[..., :-1]
    o = jnp.einsum("bnhgqs,bnshd->bnqhgd", p.astype(v.dtype), vb)
    return o.reshape(B, S, ATT_WIDTH)


def chunked_gated_scan(q, k, v, log_f):
    N, S, H, DK = q.shape
    DV = v.shape[-1]
    nc = S // HG_CHUNK

    def to_chunks(t):
        return t.reshape(N, nc, HG_CHUNK, H, t.shape[-1]).transpose(1, 0, 3, 2, 4)

    lower_tri = jnp.tril(jnp.ones((HG_CHUNK, HG_CHUNK), dtype=bool))

    def step(state, xs):
        qc, kc, vc, gc = xs
        b = jnp.cumsum(gc, axis=2)
        inter = jnp.einsum("nhcd,nhde->nhce", qc * jnp.exp(b), state)
        rel = jnp.where(lower_tri[:, :, None], b[:, :, :, None, :] - b[:, :, None, :, :], NEG_INF)
        scores = jnp.einsum("nhtd,nhsd,nhtsd->nhts", qc, kc, jnp.exp(rel))
        intra = jnp.einsum("nhts,nhse->nhte", scores, vc)
        b_last = b[:, :, -1:, :]
        new_state = (jnp.exp(b_last[:, :, 0, :])[..., None] * state
                     + jnp.einsum("nhsd,nhse->nhde", kc * jnp.exp(b_last - b), vc))
        return new_state, inter + intra

    state0 = jnp.zeros((N, H, DK, DV), jnp.float32)
    _, out = lax.scan(step, state0, (to_chunks(q), to_chunks(k), to_chunks(v), to_chunks(log_f)))
    return out.transpose(1, 0, 3, 2, 4).reshape(N, S, H, DV)


def hgrn2_bidirectional(q, f_fwd, f_bwd, i, lb_fwd, lb_bwd):
    B, S = q.shape[0], q.shape[1]

    def heads(t):
        return t.astype(jnp.float32).reshape(B, S, HG_HEADS, HG_HEAD_DIM)

    def forget(f, lb):
        f = heads(f)
        lb = lb.astype(jnp.float32).reshape(HG_HEADS, HG_HEAD_DIM)
        log_lb = jnp.log(jnp.maximum(lb, LB_FLOOR))
        log_f = jnp.logaddexp(log_lb, jnp.log1p(-lb) + jax.nn.log_sigmoid(f))
        key = (1.0 - lb) * jax.nn.sigmoid(-f)
        return log_f, key

    qh = jax.nn.silu(heads(q))
    vh = heads(i)
    lf_f, k_f = forget(f_fwd, lb_fwd)
    lf_b, k_b = forget(f_bwd, lb_bwd)
    rev = lambda t: jnp.flip(t, axis=1)
    out = chunked_gated_scan(jnp.concatenate([qh, rev(qh)], axis=0),
                             jnp.concatenate([k_f, rev(k_b)], axis=0),
                             jnp.concatenate([vh, rev(vh)], axis=0),
                             jnp.concatenate([lf_f, rev(lf_b)], axis=0))
    return out[:B] + rev(out[B:])


def hybrid_mixer(h, positions, w_in, sink, attn_gain, lb_fwd, lb_bwd, hg_gain, w_out):
    B, S = h.shape[0], h.shape[1]
    proj = jnp.einsum("bsd,de->bse", h, w_in)
    cuts = np.cumsum([ATT_WIDTH, KV_WIDTH, KV_WIDTH, HG_WIDTH, HG_WIDTH, HG_WIDTH, HG_WIDTH])
    q_a, k_a, v_a, q_h, f_fw, f_bw, i_h, g_h = jnp.split(proj, [int(t) for t in cuts], axis=-1)
    q_a = partial_rope(q_a.reshape(B, S, ATT_HEADS, ATT_HEAD_DIM), positions)
    k_a = partial_rope(k_a.reshape(B, S, ATT_KV_HEADS, ATT_HEAD_DIM), positions)
    v_a = v_a.reshape(B, S, ATT_KV_HEADS, ATT_HEAD_DIM)
    a = rms_norm(window_attention(q_a, k_a, v_a, sink), attn_gain)
    o = hgrn2_bidirectional(q_h, f_fw, f_bw, i_h, lb_fwd, lb_bwd)
    o = rms_norm(o, hg_gain) * jax.nn.silu(g_h.astype(jnp.float32).reshape(B, S, HG_HEADS, HG_HEAD_DIM))
    o = o.reshape(B, S, HG_WIDTH).astype(h.dtype)
    mixed = jnp.concatenate([a.astype(h.dtype), o], axis=-1)
    return jnp.einsum("bse,ed->bsd", mixed, w_out)


def moe_ffn(h, w_router, b_router, w_gu, b_gu, w_down, b_down):
    B, S, D = h.shape
    T = B * S
    xt = h.reshape(T, D)
    logits = (xt @ w_router).astype(jnp.float32) + b_router.astype(jnp.float32)
    top_val, top_idx = lax.top_k(logits, TOP_K)
    gates = jax.nn.softmax(top_val, axis=-1)
    TK = T * TOP_K
    flat_e = top_idx.reshape(TK).astype(jnp.int32)
    flat_tok = jnp.arange(TK, dtype=jnp.int32) // TOP_K
    flat_g = gates.reshape(TK)
    counts = jnp.zeros((N_EXPERTS,), jnp.int32).at[flat_e].add(1)
    padded = (counts + MOE_BLOCK - 1) // MOE_BLOCK * MOE_BLOCK
    start = jnp.cumsum(counts) - counts
    pend = jnp.cumsum(padded)
    pstart = pend - padded
    order = jnp.argsort(flat_e)
    se = flat_e[order]
    dest = pstart[se] + jnp.arange(TK, dtype=jnp.int32) - start[se]
    cap = TK + N_EXPERTS * MOE_BLOCK
    nblk = cap // MOE_BLOCK
    buf_tok = jnp.zeros((cap,), jnp.int32).at[dest].set(flat_tok[order])
    buf_g = jnp.zeros((cap,), jnp.float32).at[dest].set(flat_g[order])
    blk_expert = jnp.clip(jnp.searchsorted(pend, jnp.arange(nblk, dtype=jnp.int32) * MOE_BLOCK,
                                           side="right"), 0, N_EXPERTS - 1).astype(jnp.int32)
    xs = xt[buf_tok].reshape(nblk, MOE_BLOCK, D)

    def expert_block(args):
        xb, e = args
        hu = xb @ w_gu[e] + b_gu[e]
        glu = jnp.minimum(hu[:, :D_FF], SWIGLU_LIMIT)
        lin = jnp.clip(hu[:, D_FF:], -SWIGLU_LIMIT, SWIGLU_LIMIT)
        act = glu * jax.nn.sigmoid(SWIGLU_ALPHA * glu) * (lin + 1.0)
        return act @ w_down[e] + b_down[e]

    ys = lax.map(expert_block, (xs, blk_expert)).reshape(cap, D)
    out = jnp.zeros((T, D), ys.dtype).at[buf_tok].add(ys * buf_g[:, None].astype(ys.dtype))
    return out.reshape(B, S, D).astype(h.dtype)


def setup_inputs(seed: int = 0) -> dict:
    key = jax.random.key(seed)
    ks = jax.random.split(key, 20)
    f32 = jnp.float32

    def nrm(k, shape, scale):
        return scale * jax.random.normal(k, shape, f32)

    def gain(k, shape):
        return 1.0 + 0.02 * jax.random.normal(k, shape, f32)

    return {
        "x": jax.random.normal(ks[0], (BATCH, SEQ, D_MODEL), f32),
        "c": jax.random.normal(ks[1], (BATCH, D_MODEL), f32),
        "positions": jnp.broadcast_to(jnp.arange(SEQ, dtype=jnp.int32), (BATCH, SEQ)),
        "w_ada": nrm(ks[2], (DEPTH, D_MODEL, N_MOD * D_MODEL), 0.5 * D_MODEL ** -0.5),
        "b_ada": nrm(ks[3], (DEPTH, N_MOD * D_MODEL), 0.02),
        "norm1": gain(ks[4], (DEPTH, D_MODEL)),
        "w_in": nrm(ks[5], (DEPTH, D_MODEL, IN_WIDTH), D_MODEL ** -0.5),
        "attn_sink": nrm(ks[6], (DEPTH, ATT_HEADS), 0.5),
        "attn_norm": gain(ks[7], (DEPTH, ATT_WIDTH)),
        "hg_lb_logits": nrm(ks[8], (DEPTH, 2, HG_WIDTH), 0.5),
        "hg_norm": gain(ks[9], (DEPTH, HG_HEAD_DIM)),
        "w_out": nrm(ks[10], (DEPTH, MIX_WIDTH, D_MODEL), MIX_WIDTH ** -0.5),
        "norm2": gain(ks[11], (DEPTH, D_MODEL)),
        "w_router": nrm(ks[12], (DEPTH, D_MODEL, N_EXPERTS), D_MODEL ** -0.5),
        "b_router": nrm(ks[13], (DEPTH, N_EXPERTS), 0.01),
        "w_gu": nrm(ks[14], (DEPTH, N_EXPERTS, D_MODEL, 2 * D_FF), D_MODEL ** -0.5),
        "b_gu": nrm(ks[15], (DEPTH, N_EXPERTS, 2 * D_FF), 0.02),
        "w_down": nrm(ks[16], (DEPTH, N_EXPERTS, D_FF, D_MODEL), D_FF ** -0.5),
        "b_down": nrm(ks[17], (DEPTH, N_EXPERTS, D_MODEL), 0.02),
        "final_norm": gain(ks[18], (D_MODEL,)),
    }


def reference(x, c, positions, w_ada, b_ada, norm1, w_in, attn_sink, attn_norm, hg_lb_logits,
              hg_norm, w_out, norm2, w_router, b_router, w_gu, b_gu, w_down, b_down, final_norm):
    cond = jax.nn.silu(c)
    lb_p = jax.nn.softmax(hg_lb_logits.astype(jnp.float32), axis=0)
    lower = jnp.cumsum(lb_p, axis=0) - lb_p[0]
    for l in range(DEPTH):
        mod = cond @ w_ada[l] + b_ada[l]
        sh1, sc1, g1, sh2, sc2, g2 = jnp.split(mod[:, None, :], N_MOD, axis=-1)
        h = rms_norm(x, norm1[l]) * (1.0 + sc1) + sh1
        x = x + g1 * hybrid_mixer(h, positions, w_in[l], attn_sink[l], attn_norm[l],
                                  lower[l, 0], lower[l, 1], hg_norm[l], w_out[l])
        h = rms_norm(x, norm2[l]) * (1.0 + sc2) + sh2
        x = x + g2 * moe_ffn(h, w_router[l], b_router[l], w_gu[l], b_gu[l], w_down[l], b_down[l])
    return rms_norm(x, final_norm)
```

```python
import contextlib
import numpy as np
import ml_dtypes
import concourse.bass as bass
import concourse.mybir as mybir
from concourse.bass_utils import run_bass_kernel_spmd

F32 = mybir.dt.float32
BF16 = mybir.dt.bfloat16
I32 = mybir.dt.int32
AF = mybir.ActivationFunctionType
ALU = mybir.AluOpType

D = 1024
NIN = 3328
EPS = 1e-6
PE, ACT, DVE, POOL, SP = "pe", "act", "dve", "pool", "sp"
ENGS = [PE, ACT, DVE, POOL, SP]
SEM_CAP = 30000


class Buf:
    __slots__ = ("w", "r", "excl")

    def __init__(self, excl=False):
        self.w = None
        self.r = []
        self.excl = excl


class Ins:
    __slots__ = ("id", "eng", "fn", "deps", "is_dma", "dma_sem", "dma_val", "signal", "sig_sem", "sig_val")


class Prog:
    def __init__(self, nc):
        self.nc = nc
        self.ins = []
        self.q = {e: [] for e in ENGS}
        self.dma_cnt = {}
        self.last_dma = {}
        self.fence_ids = set()
        self.fence_epoch = 0
        self.eng_epoch = {e: 0 for e in ENGS}

    def fence(self):
        ids = set(self.last_dma.values())
        for e in ENGS:
            if self.q[e]:
                ids.add(self.q[e][-1].id)
        self.fence_ids = ids
        self.fence_epoch += 1

    def _rec(self, eng, fn, reads, writes):
        i = Ins()
        i.id = len(self.ins)
        i.eng = eng
        i.fn = fn
        i.is_dma = False
        i.signal = False
        i.dma_sem = None
        deps = set()
        if self.eng_epoch[eng] != self.fence_epoch:
            deps.update(self.fence_ids)
            self.eng_epoch[eng] = self.fence_epoch
        for b in reads:
            if b.w is not None:
                deps.add(b.w)
            if b.excl:
                deps.update(r for r in b.r if self.ins[r].eng != eng)
        for b in writes:
            if b.w is not None:
                deps.add(b.w)
            deps.update(b.r)
        if eng == PE:
            deps = {d for d in deps if self.ins[d].eng != PE}
        i.deps = deps
        for b in reads:
            b.r.append(i.id)
        for b in writes:
            b.w = i.id
            b.r = []
        self.ins.append(i)
        self.q[eng].append(i)
        return i.id

    def op(self, eng, fn, reads=(), writes=()):
        return self._rec(eng, fn, list(reads), list(writes))

    def dma(self, eng, fn, slot, reads=(), writes=()):
        iid = self._rec(eng, fn, list(reads), list(writes))
        i = self.ins[iid]
        if slot in self.last_dma:
            i.deps.add(self.last_dma[slot])
        self.last_dma[slot] = iid
        i.is_dma = True
        self.dma_cnt[slot] = self.dma_cnt.get(slot, 0) + 16
        i.dma_sem = slot
        i.dma_val = self.dma_cnt[slot]
        return iid

    def emit(self, final_waits=()):
        nc = self.nc
        ins = self.ins
        for i in ins:
            for d in i.deps:
                if not ins[d].is_dma:
                    ins[d].signal = True
        nsig = {e: 0 for e in ENGS}
        for e in ENGS:
            for i in self.q[e]:
                if i.signal and not i.is_dma:
                    n = nsig[e]
                    i.sig_sem = (e, n // SEM_CAP)
                    i.sig_val = n % SEM_CAP + 1
                    nsig[e] = n + 1
        keys = set()
        for i in ins:
            if i.is_dma:
                keys.add(("dma", i.dma_sem))
            elif i.signal:
                keys.add(i.sig_sem)
        keys = sorted(keys, key=str)
        self.n_sems = len(keys)
        with contextlib.ExitStack() as st:
            sems = {}
            for k in keys:
                sems[k] = st.enter_context(nc.semaphore("s%d" % len(sems)))
            block = st.enter_context(nc.Block())
            engobj = {PE: "tensor", ACT: "scalar", DVE: "vector", POOL: "gpsimd", SP: "sync"}

            def make(e):
                def body(eng):
                    known = {}
                    for i in self.q[e]:
                        need = {}
                        for d in i.deps:
                            di = ins[d]
                            if di.is_dma:
                                k, v = ("dma", di.dma_sem), di.dma_val
                            else:
                                k, v = di.sig_sem, di.sig_val
                            if v > need.get(k, 0):
                                need[k] = v
                        for k, v in need.items():
                            if known.get(k, 0) < v:
                                eng.wait_ge(sems[k], v)
                                known[k] = v
                        r = i.fn(eng)
                        if i.is_dma:
                            r.then_inc(sems[("dma", i.dma_sem)], 16)
                        elif i.signal:
                            r.then_inc(sems[i.sig_sem], 1)
                    if e == SP:
                        for d in final_waits:
                            di = ins[d]
                            eng.wait_ge(sems[("dma", di.dma_sem)], di.dma_val)
                return body

            for e in ENGS:
                if self.q[e] or e == SP:
                    getattr(block, engobj[e])(make(e))


class Arena:
    def __init__(self, t, nwords):
        self.t = t
        self.cap = nwords
        self.top = 0
        self.peak = 0
        self.prog = None

    def alloc(self, shape, dtype=F32):
        n = int(np.prod(shape))
        words = n if dtype in (F32, I32) else (n + 1) // 2
        words = (words + 15) // 16 * 16
        off = self.top
        self.top += words
        self.peak = max(self.peak, self.top)
        assert self.top <= self.cap, ("SBUF arena overflow", self.top, self.cap)
        ap = self.t[:, off:off + (n if dtype in (F32, I32) else (n + 1) // 2)]
        if dtype != F32:
            ap = ap.bitcast(dtype)
        if len(shape) == 2:
            ap = ap.rearrange("p (a b) -> p a b", a=shape[0])
        elif len(shape) == 3:
            ap = ap.rearrange("p (a b c) -> p a b c", a=shape[0], b=shape[1])
        elif len(shape) == 4:
            ap = ap.rearrange("p (a b c d) -> p a b c d", a=shape[0], b=shape[1], c=shape[2])
        return ap, Buf()

    def mark(self):
        return self.top

    def release(self, m):
        self.top = m
        if self.prog is not None:
            self.prog.fence()


def _consts():
    bf = ml_dtypes.bfloat16
    p = np.arange(128)
    c = {}
    c["ident_bf"] = np.eye(128, dtype=np.float32).astype(bf)
    c["ident_f"] = np.eye(128, dtype=np.float32)
    c["ones_bf"] = np.ones((128, 128), np.float32).astype(bf)
    c["ones_f"] = np.ones((128, 128), np.float32)
    same = (p[:, None] // 32) == (p[None, :] // 32)
    mf = (same & (p[:, None] <= p[None, :])).astype(np.float32)
    mb = (same & (p[:, None] >= p[None, :])).astype(np.float32)
    c["hmask"] = np.stack([np.tile(mf[:, None, :], (1, 4, 1)), np.tile(mb[:, None, :], (1, 4, 1))], 1).astype(bf)
    ml = (p[:, None] >= p[None, :]).astype(np.float32)
    mr = (p[:, None] <= p[None, :]).astype(np.float32)
    c["amask"] = np.stack([np.tile(ml[:, None, :], (1, 4, 1)), np.tile(mr[:, None, :], (1, 4, 1))], 1).astype(bf)
    c["bm4"] = (p[:, None] // 32 == np.arange(4)[None, :]).astype(np.float32)
    c["rmask"] = np.tile((np.arange(512) % 32 != 0).astype(np.float32)[None, :], (128, 1))
    invf = (500000.0 ** (-(np.arange(8, dtype=np.float32) * 2.0 / 16))).astype(np.float32)
    rp = np.zeros((128, 2), np.float32)
    for base in (0, 64):
        rp[base:base + 16, 0] = np.concatenate([invf, invf])
        rp[base:base + 16, 1] = np.concatenate([-np.ones(8), np.ones(8)])
    c["ropec"] = rp
    return c


CONST_SPECS = [("ident_bf", [128, 128], BF16), ("ident_f", [128, 128], F32), ("ones_bf", [128, 128], BF16),
               ("ones_f", [128, 128], F32), ("hmask", [128, 2, 4, 128], BF16), ("amask", [128, 2, 4, 128], BF16),
               ("bm4", [128, 4], F32), ("rmask", [128, 512], F32), ("ropec", [128, 2], F32)]


def build(S, E, L, stop=None):
    NT = S // 128
    NQ = S // 512
    NCH = S // 32
    nc = bass.Bass("TRN2", target_bir_lowering=False)

    def din(name, shape, dt=F32):
        return nc.dram_tensor(name, list(shape), dt, kind="ExternalInput").ap()

    x_d = din("x", [S, D])
    cT_d = din("cT", [128, 8])
    pos_d = din("pos16", [128, S], I32)
    wada_d = din("w_ada", [L, D, 6 * D])
    bada_d = din("b_ada", [L, 1, 6 * D])
    n1_d = din("norm1T", [L, 128, 8])
    n2_d = din("norm2T", [L, 128, 8])
    win_d = din("w_in", [L, D, NIN])
    sink_d = din("sinkb", [L, 128, 8])
    an_d = din("anormb", [L, 128, 512])
    lbl_d = din("lblT", [128, L, 8])
    hgn_d = din("hgnT", [128, L])
    wout_d = din("w_out", [L, D, D])
    wr_d = din("w_router", [L, D, E])
    br_d = din("brb", [L, 128, E])
    wgu_d = din("w_gu", [L, E, D, 2 * D])
    bgu_d = din("bguT", [L, 128, E, 16])
    wd_d = din("w_down", [L, E, D, D])
    bd_d = din("b_down", [L, E, D])
    fn_d = din("fnb", [128, D])
    cd = {n: din("c_" + n, sh, dt) for n, sh, dt in CONST_SPECS}
    y_d = nc.dram_tensor("y", [S, D], F32, kind="ExternalOutput").ap()

    st = contextlib.ExitStack()
    with st:
        NW = 53200
        arena_t = st.enter_context(nc.sbuf_tensor("arena", [128, NW], F32))
        A = Arena(arena_t, NW)
        banks = []
        for i in range(8):
            t = st.enter_context(nc.psum_tensor("pb%d" % i, [128, 512], F32))
            banks.append((t, Buf(excl=True)))
        bctr = [0]

        def nb():
            b = banks[bctr[0] % 8]
            bctr[0] += 1
            return b

        P = Prog(nc)
        A.prog = P

        def mm(out, lhsT, rhs, R, W, start=True, stop=True, skip=False):
            P.op(PE, lambda e: e.matmul(out, lhsT=lhsT, rhs=rhs, start=start, stop=stop, skip_group_check=skip), R, W)

        def tr(out, in_, ident, R, W):
            P.op(PE, lambda e: e.transpose(out, in_, ident), R, W)

        def act(out, in_, func, R, W, scale=1.0, bias=None, accum=None):
            def f(e):
                kw = {}
                if bias is not None:
                    kw["bias"] = bias
                if accum is not None:
                    kw["accum_out"] = accum
                return e.activation(out=out, in_=in_, func=func, scale=scale, **kw)
            P.op(ACT, f, R, W)

        def ts(out, in0, s1, s2, op0, op1, R, W):
            if s2 is None:
                P.op(DVE, lambda e: e.tensor_scalar(out=out, in0=in0, scalar1=s1, scalar2=None, op0=op0), R, W)
            else:
                P.op(DVE, lambda e: e.tensor_scalar(out=out, in0=in0, scalar1=s1, scalar2=s2, op0=op0, op1=op1), R, W)

        def tt(out, in0, in1, op, R, W):
            P.op(DVE, lambda e: e.tensor_tensor(out=out, in0=in0, in1=in1, op=op), R, W)

        def stt(out, in0, sc, in1, op0, op1, R, W, accum=None):
            if accum is None:
                P.op(DVE, lambda e: e.scalar_tensor_tensor(out=out, in0=in0, scalar=sc, in1=in1, op0=op0, op1=op1), R, W)
            else:
                P.op(DVE, lambda e: e.scalar_tensor_tensor(out=out, in0=in0, scalar=sc, in1=in1, op0=op0, op1=op1, accum_out=accum), R, W)

        def cp(out, in_, R, W):
            P.op(DVE, lambda e: e.tensor_copy(out=out, in_=in_), R, W)

        def recip(out, in_, R, W):
            P.op(DVE, lambda e: e.reciprocal(out=out, in_=in_), R, W)

        def dma(eng, out, in_, slot, R, W, ncok=False):
            if ncok:
                P.dma(eng, lambda e: e.dma_start(out=out, in_=in_, allow_slow_non_contiguous=True), slot, R, W)
            else:
                P.dma(eng, lambda e: e.dma_start(out=out, in_=in_), slot, R, W)

        def rsqrt_chain(dst, src, scale, R_, W_, tmpb):
            ts(dst, src, scale, EPS, ALU.mult, ALU.add, R_, W_)
            act(dst, dst, AF.Sqrt, W_, W_)
            recip(dst, dst, W_, W_)

        X, _ = A.alloc([NT, D])
        xb = [Buf() for _ in range(NT)]
        K = {}
        for n, sh, dt in CONST_SPECS:
            K[n], kb_ = A.alloc(sh[1:], dt)
            K[n + "_b"] = kb_
        for n, sh, dt in CONST_SPECS:
            np_ = sh[0]
            dma(SP, K[n][0:np_], cd[n], "const", [], [K[n + "_b"]])
        ident_bf, ident_f, ones_bf, ones_f = K["ident_bf"], K["ident_f"], K["ones_bf"], K["ones_f"]
        CB = [K[n + "_b"] for n, _, _ in CONST_SPECS]
        condT, condb = A.alloc([8])
        lbT, lbb = A.alloc([L, 8])
        omlT, _ = A.alloc([L, 8])
        lbm1T, _ = A.alloc([L, 8])
        hgnT, hgnb = A.alloc([L])
        gbc, gbcb = A.alloc([2, D])
        modT, modTb = A.alloc([4, 8])
        hT, _ = A.alloc([8, S], BF16)
        hb = [Buf() for _ in range(NQ)]
        ropeC, ropeb = A.alloc([S])
        ropeS, _ = A.alloc([S])

        dma(SP, X, x_d.rearrange("(i p) d -> p i d", p=128), "x", [], xb)
        dma(SP, condT, cT_d, "small", [], [condb])
        dma(SP, hgnT, hgn_d, "small", [], [hgnb])

        m0 = A.mark()
        t8, t8b = A.alloc([8])
        act(t8, condT, AF.Sigmoid, [condb], [t8b])
        tt(condT, condT, t8, ALU.mult, [condb, t8b], [condb])
        lraw, lrawb = A.alloc([L, 8])
        lsum, lsumb = A.alloc([8])
        dma(SP, lraw, lbl_d, "small", [], [lrawb])
        act(lraw, lraw, AF.Exp, [lrawb], [lrawb])
        cp(lsum, lraw[:, 0, :], [lrawb], [lsumb])
        for l in range(1, L):
            tt(lsum, lsum, lraw[:, l, :], ALU.add, [lsumb, lrawb], [lsumb])
        recip(lsum, lsum, [lsumb], [lsumb])
        P.op(DVE, lambda e: e.memset(lbT[:, 0, :], 0.0), [], [lbb])
        for l in range(1, L):
            tt(t8, lraw[:, l, :], lsum, ALU.mult, [lrawb, lsumb], [t8b])
            tt(lbT[:, l, :], lbT[:, l - 1, :], t8, ALU.add, [lbb, t8b], [lbb])
        ts(omlT, lbT, -1.0, 1.0, ALU.mult, ALU.add, [lbb], [lbb])
        ts(lbm1T, lbT, -1.0, None, ALU.add, None, [lbb], [lbb])
        TWO_PI = float(2 * np.pi)
        posi, posb = A.alloc([S], I32)
        ang, angb = A.alloc([S])
        rr, rrb = A.alloc([S])
        ni, nib = A.alloc([S], I32)
        dma(SP, posi, pos_d, "small", [], [posb])
        rc = K["ropec"]
        cp(ang, posi, [posb], [angb])
        ts(ang, ang, rc[:, 0:1], None, ALU.mult, None, [angb, K["ropec_b"]], [angb])
        ts(rr, ang, 1.0 / TWO_PI, None, ALU.mult, None, [angb], [rrb])
        cp(ni, rr, [rrb], [nib])
        cp(rr, ni, [nib], [rrb])
        stt(ang, rr, -TWO_PI, ang, ALU.mult, ALU.add, [rrb, angb], [angb])

        def wrap_sin(dst, shift, dstb):
            ts(rr, ang, shift, None, ALU.add, None, [angb], [rrb])
            ts(ni.bitcast(F32), rr, float(np.pi), -TWO_PI, ALU.is_gt, ALU.mult, [rrb], [nib])
            tt(rr, rr, ni.bitcast(F32), ALU.add, [rrb, nib], [rrb])
            ts(ni.bitcast(F32), rr, float(-np.pi), TWO_PI, ALU.is_lt, ALU.mult, [rrb], [nib])
            tt(rr, rr, ni.bitcast(F32), ALU.add, [rrb, nib], [rrb])
            act(dst, rr, AF.Sin, [rrb], [dstb])

        wrap_sin(ropeC, float(np.pi / 2), ropeb)
        wrap_sin(ropeS, 0.0, ropeb)
        ts(ropeS, ropeS, rc[:, 1:2], None, ALU.mult, None, [ropeb, K["ropec_b"]], [ropeb])
        A.release(m0)

        def norm_phase(l, which, router=None):
            m = A.mark()
            ss, ssb = A.alloc([NT])
            junk, junkb = A.alloc([D])
            xn_dt = F32 if router is not None else BF16
            xn = [A.alloc([D], xn_dt) for _ in range(4)]
            for i in range(NT):
                act(junk, X[:, i, :], AF.Square, [xb[i]], [junkb, ssb], accum=ss[:, i:i + 1])
            rsqrt_chain(ss, ss, 1.0 / D, [ssb], [ssb], None)
            aT = modT[:, 2 * which, :]
            bT = modT[:, 2 * which + 1, :]
            if router is not None:
                hn, hnb = A.alloc([8, 512])
            for q in range(NQ):
                for ii in range(4):
                    i = q * 4 + ii
                    ts(xn[ii][0], X[:, i, :], ss[:, i:i + 1], None, ALU.mult, None, [xb[i], ssb], [xn[ii][1]])
                for k in range(8):
                    bk, bkb = nb()
                    if router is None:
                        bv = bk.bitcast(BF16)
                        for ii in range(4):
                            tr(bv[:, ii * 128:(ii + 1) * 128], xn[ii][0][:, k * 128:(k + 1) * 128], ident_bf, [xn[ii][1]] + CB, [bkb])
                        ts(hT[:, k, q * 512:(q + 1) * 512], bv[:, 0:512], aT[:, k:k + 1], bT[:, k:k + 1], ALU.mult, ALU.add,
                           [bkb, modTb], [hb[q]])
                    else:
                        for ii in range(4):
                            tr(bk[:, ii * 128:(ii + 1) * 128], xn[ii][0][:, k * 128:(k + 1) * 128], ident_f, [xn[ii][1]] + CB, [bkb])
                        ts(hn[:, k, :], bk[:, 0:512], aT[:, k:k + 1], bT[:, k:k + 1], ALU.mult, ALU.add, [bkb, modTb], [hnb])
                        act(hT[:, k, q * 512:(q + 1) * 512], hn[:, k, :], AF.Copy, [hnb], [hb[q]])
                if router is not None:
                    router(q, hn, hnb)
            A.release(m)

        def x_update(i, half, psum, psb, gidx, gate=None, gate_b=None):
            tmp, tmpb = xtmp[0]
            xtc[0] += 1
            sl = slice(half * 512, (half + 1) * 512)
            tt(tmp, psum, gbc[:, gidx, sl], ALU.mult, [psb, gbcb], [tmpb])
            if gate is None:
                tt(X[:, i, sl], X[:, i, sl], tmp, ALU.add, [xb[i], tmpb], [xb[i]])
            else:
                stt(X[:, i, sl], tmp, gate, X[:, i, sl], ALU.mult, ALU.add, [tmpb, gate_b, xb[i]], [xb[i]])

        xtmp = [A.alloc([512]) for _ in range(1)]
        xtc = [0]
        win_v = [win_d[l].rearrange("(k p) n -> p k n", p=128) for l in range(L)]
        wout_v = [wout_d[l].rearrange("(k p) n -> p k n", p=128) for l in range(L)]

        for l in range(L if stop != "pro" else 0):
            m = A.mark()
            modrow, modrowb = A.alloc([6 * D])
            badar = [A.alloc([512]) for _ in range(2)]
            wblk = [A.alloc([8, 512]) for _ in range(2)]
            n12, n12b = A.alloc([2, 8])
            dma(SP, n12[:, 0, :], n1_d[l], "small", [], [n12b])
            dma(SP, n12[:, 1, :], n2_d[l], "small", [], [n12b])
            wav = wada_d[l].rearrange("(k p) n -> p k n", p=128)
            for j in range(12):
                wb_, wbb = wblk[j % 2]
                dma(SP, wb_, wav[:, :, j * 512:(j + 1) * 512], ("wada", j % 2), [], [wbb])
                bd_, bdb_ = badar[j % 2]
                dma(SP, bd_[0:1], bada_d[l][:, j * 512:(j + 1) * 512], ("bada", j % 2), [], [bdb_])
                bk, bkb = nb()
                for k in range(8):
                    mm(bk[0:1, :], condT[:, k:k + 1], wb_[:, k, :], [condb, wbb], [bkb], start=(k == 0), stop=(k == 7))
                tt(modrow[0:1, j * 512:(j + 1) * 512], bk[0:1, :], bd_[0:1, :], ALU.add,
                   [bkb, bdb_], [modrowb])
            for gi, v in enumerate((2, 5)):
                for hf in range(2):
                    bk, bkb = nb()
                    mm(bk[:, :], ones_f[0:1, 0:128], modrow[0:1, v * D + hf * 512: v * D + (hf + 1) * 512], [modrowb] + CB, [bkb])
                    cp(gbc[:, gi, hf * 512:(hf + 1) * 512], bk[:, :], [bkb], [gbcb])
            bk, bkb = nb()
            for vi, v in enumerate((1, 0, 4, 3)):
                for k in range(8):
                    col = (vi * 8 + k) * 2
                    mm(bk[:, col:col + 2], modrow[0:1, v * D + k * 128: v * D + (k + 1) * 128], ones_f[0:1, 0:2], [modrowb] + CB, [bkb])
            bkv = bk[:, 0:64].rearrange("p (v k two) -> p v k two", v=4, k=8)
            cp(modT, bkv[:, :, :, 0], [bkb], [modTb])
            for w_ in range(2):
                ts(modT[:, 2 * w_, :], modT[:, 2 * w_, :], 1.0, None, ALU.add, None, [modTb], [modTb])
                tt(modT[:, 2 * w_, :], modT[:, 2 * w_, :], n12[:, w_, :], ALU.mult, [modTb, n12b], [modTb])
            A.release(m)

            if stop == "ada":
                continue
            norm_phase(l, 0)
            if stop == "n1":
                continue

            for hh in range(4):
                m = A.mark()
                Wh, Whb = A.alloc([8, 5, 128], BF16)
                woh, wohb = A.alloc([D], BF16)
                for j, c0 in enumerate((768, 1280, 1792, 2304, 2816)):
                    dma(POOL, Wh[:, :, j, :], win_v[l][:, :, c0 + hh * 128: c0 + (hh + 1) * 128], "wh", [], [Whb])
                dma(POOL, woh, wout_d[l][512 + hh * 128: 512 + (hh + 1) * 128, :], "wh", [], [wohb])
                qside = [A.alloc([S], BF16) for _ in range(2)]
                kside = [A.alloc([S], BF16) for _ in range(2)]
                ktok = [A.alloc([NT, 128], BF16) for _ in range(2)]
                vtok, vtokb = A.alloc([NT, 128], BF16)
                Gt = [A.alloc([NCH]) for _ in range(2)]
                oacc, oaccb = A.alloc([S])
                T = [A.alloc([512]) for _ in range(8)]
                for q in range(NQ):
                    qs = slice(q * 512, (q + 1) * 512)
                    bk, bkb = nb()
                    for k in range(8):
                        mm(bk[:, :], Wh[:, k, 0, :], hT[:, k, qs], [Whb, hb[q]], [bkb], start=(k == 0), stop=(k == 7))
                    act(T[0][0], bk[:, :], AF.Sigmoid, [bkb], [T[0][1]])
                    tt(T[1][0], bk[:, :], T[0][0], ALU.mult, [bkb, T[0][1]], [T[1][1]])
                    for d in range(2):
                        li = d * 4 + hh
                        bk, bkb = nb()
                        for k in range(8):
                            mm(bk[:, :], Wh[:, k, 1 + d, :], hT[:, k, qs], [Whb, hb[q]], [bkb], start=(k == 0), stop=(k == 7))
                        act(T[2][0], bk[:, :], AF.Sigmoid, [bkb], [T[2][1]])
                        ts(T[3][0], T[2][0], omlT[:, l, li:li + 1], lbT[:, l, li:li + 1], ALU.mult, ALU.add, [T[2][1], lbb], [T[3][1]])
                        ts(T[4][0], T[2][0], lbm1T[:, l, li:li + 1], omlT[:, l, li:li + 1], ALU.mult, ALU.add, [T[2][1], lbb], [T[4][1]])
                        act(T[3][0], T[3][0], AF.Ln, [T[3][1]], [T[3][1]])
                        P.op(DVE, lambda e, o=T[5][0], a=K["rmask"], b_=T[3][0]: e.tensor_tensor_scan(
                            out=o, data0=a, data1=b_, initial=0.0, op0=ALU.mult, op1=ALU.add), [T[3][1], K["rmask_b"]], [T[5][1]])
                        bend = T[5][0].rearrange("p (c j) -> p c j", j=32)[:, :, 31]
                        if d == 0:
                            act(T[6][0], T[5][0], AF.Exp, [T[5][1]], [T[6][1]])
                            act(T[7][0], T[5][0], AF.Exp, [T[5][1]], [T[7][1]], scale=-1.0)
                        else:
                            tt(T[3][0], T[5][0], T[3][0], ALU.subtract, [T[5][1], T[3][1]], [T[3][1]])
                            act(T[6][0], T[3][0], AF.Exp, [T[3][1]], [T[6][1]], scale=-1.0)
                            act(T[7][0], T[3][0], AF.Exp, [T[3][1]], [T[7][1]])
                        act(Gt[d][0][:, q * 16:(q + 1) * 16], bend, AF.Exp, [T[5][1]], [Gt[d][1]])
                        tt(qside[d][0][:, qs], T[1][0], T[6][0], ALU.mult, [T[1][1], T[6][1]], [qside[d][1]])
                        tt(kside[d][0][:, qs], T[4][0], T[7][0], ALU.mult, [T[4][1], T[7][1]], [kside[d][1]])
                        bk, bkb = nb()
                        bv = bk.bitcast(BF16)
                        for ii in range(4):
                            tr(bv[:, ii * 128:(ii + 1) * 128], kside[d][0][:, q * 512 + ii * 128: q * 512 + (ii + 1) * 128], ident_bf,
                               [kside[d][1]] + CB, [bkb])
                        cp(ktok[d][0][:, q * 4:(q + 1) * 4, :], bv[:, 0:512].rearrange("p (i c) -> p i c", i=4), [bkb], [ktok[d][1]])
                    bk, bkb = nb()
                    for ii in range(4):
                        tsl = slice(q * 512 + ii * 128, q * 512 + (ii + 1) * 128)
                        for k in range(8):
                            mm(bk[:, ii * 128:(ii + 1) * 128], hT[:, k, tsl], Wh[:, k, 3, :], [Whb, hb[q]], [bkb], start=(k == 0), stop=(k == 7))
                    cp(vtok[:, q * 4:(q + 1) * 4, :], bk[:, :].rearrange("p (i c) -> p i c", i=4), [bkb], [vtokb])
                Wst = [A.alloc([128]) for _ in range(2)]
                Ub = [A.alloc([16, 128], BF16) for _ in range(1)]
                Vm = [A.alloc([4, 4, 128], BF16) for _ in range(1)]
                Abd = [A.alloc([4, 128], BF16) for _ in range(1)]
                onT = [A.alloc([512], BF16) for _ in range(1)]
                wcnt = 0
                have_prev = False
                for d in range(2):
                    have_prev = False
                    qorder = range(NQ) if d == 0 else range(NQ - 1, -1, -1)
                    for qi_, q in enumerate(qorder):
                        qs = slice(q * 512, (q + 1) * 512)
                        vm, vmb = Vm[0]
                        ub, ubb = Ub[0]
                        abd, abdb = Abd[0]
                        for ii in range(4):
                            for r in range(4):
                                ts(vm[:, ii, r, :], vtok[:, q * 4 + ii, :], K["bm4"][:, r:r + 1], None, ALU.mult, None,
                                   [vtokb, K["bm4_b"]], [vmb])
                        zb = []
                        for ii in range(4):
                            bk, bkb = nb()
                            mm(bk[:, :], ktok[d][0][:, q * 4 + ii, :], vm[:, ii, :, :], [ktok[d][1], vmb], [bkb])
                            zb.append((bk, bkb))
                        corder = range(16) if d == 0 else range(15, -1, -1)
                        uvalid = [False] * 16
                        for cl in corder:
                            c = q * 16 + cl
                            zc = zb[cl // 4][0][:, (cl % 4) * 128:(cl % 4 + 1) * 128]
                            zcb = zb[cl // 4][1]
                            wnew, wnewb = Wst[wcnt % 2]
                            wold, woldb = Wst[(wcnt + 1) % 2]
                            if not have_prev:
                                cp(wnew, zc, [zcb], [wnewb])
                            else:
                                gidx = (c - 1) if d == 0 else c
                                gsc = Gt[d][0][:, gidx:gidx + 1]
                                act(ub[:, cl, :], wold, AF.Copy, [woldb, Gt[d][1]], [ubb], scale=gsc)
                                uvalid[cl] = True
                                stt(wnew, wold, gsc, zc, ALU.mult, ALU.add, [woldb, Gt[d][1], zcb], [wnewb])
                            have_prev = True
                            wcnt += 1
                        bk, bkb = nb()
                        for ii in range(4):
                            tsl = slice(q * 512 + ii * 128, q * 512 + (ii + 1) * 128)
                            mm(bk[:, ii * 128:(ii + 1) * 128], kside[d][0][:, tsl], qside[d][0][:, tsl], [kside[d][1], qside[d][1]], [bkb])
                        tt(abd, bk[:, :].rearrange("p (i c) -> p i c", i=4), K["hmask"][:, d, :, :], ALU.mult, [bkb, K["hmask_b"]], [abdb])
                        ob, obb = nb()
                        first = True
                        for ii in range(4):
                            mm(ob[:, ii * 128:(ii + 1) * 128], vtok[:, q * 4 + ii, :], abd[:, ii, :], [vtokb, abdb], [obb],
                               start=first, stop=False, skip=True)
                            first = False
                        nl = sum(uvalid)
                        cnt = 0
                        for cl in range(16):
                            if not uvalid[cl]:
                                continue
                            cnt += 1
                            mm(ob[:, cl * 32:(cl + 1) * 32], ub[:, cl, :], qside[d][0][:, q * 512 + cl * 32: q * 512 + (cl + 1) * 32],
                               [ubb, qside[d][1]], [obb], start=False, stop=(cnt == nl), skip=True)
                        if d == 0:
                            cp(oacc[:, qs], ob[:, :], [obb], [oaccb])
                        else:
                            o_, o_b = T[0]
                            tt(o_, ob[:, :], oacc[:, qs], ALU.add, [obb, oaccb], [o_b])
                            act(T[1][0].bitcast(BF16)[:, 0:512], o_, AF.Square, [o_b], [T[1][1]])
                            bk, bkb = nb()
                            mm(bk[:, :], ones_bf, T[1][0].bitcast(BF16)[:, 0:512], [T[1][1]] + CB, [bkb])
                            ts(T[2][0], bk[:, :], 1.0 / 128, EPS, ALU.mult, ALU.add, [bkb], [T[2][1]])
                            act(T[2][0], T[2][0], AF.Sqrt, [T[2][1]], [T[2][1]])
                            recip(T[2][0], T[2][0], [T[2][1]], [T[2][1]])
                            stt(T[3][0], o_, hgnT[:, l:l + 1], T[2][0], ALU.mult, ALU.mult, [o_b, hgnb, T[2][1]], [T[3][1]])
                            bk, bkb = nb()
                            for k in range(8):
                                mm(bk[:, :], Wh[:, k, 4, :], hT[:, k, qs], [Whb, hb[q]], [bkb], start=(k == 0), stop=(k == 7))
                            act(T[4][0], bk[:, :], AF.Sigmoid, [bkb], [T[4][1]])
                            tt(T[4][0], bk[:, :], T[4][0], ALU.mult, [bkb, T[4][1]], [T[4][1]])
                            on_, onb_ = onT[0]
                            tt(on_, T[3][0], T[4][0], ALU.mult, [T[3][1], T[4][1]], [onb_])
                            for ii in range(4):
                                for hf in range(2):
                                    bk, bkb = nb()
                                    mm(bk[:, :], on_[:, ii * 128:(ii + 1) * 128], woh[:, hf * 512:(hf + 1) * 512], [onb_, wohb], [bkb])
                                    x_update(q * 4 + ii, hf, bk[:, :], bkb, 0)
                A.release(m)

            if stop == "hg":
                continue
            m = A.mark()
            lim = int(stop[2:]) if (stop or "").startswith("at") else 99
            Wq2, Wqb = A.alloc([8, 512], BF16)
            Wqs2, Wqsb = A.alloc([8, 512], BF16)
            woa, woab = A.alloc([4, D], BF16)
            kz = [[A.alloc([S], BF16) for _ in range(2)] for _ in range(2)]
            Va, Vab = A.alloc([NT, 2, 66], BF16)
            esink, esinkb = A.alloc([8])
            anb, anbb = A.alloc([512])
            r1 = [A.alloc([128]) for _ in range(2)]
            r2 = [A.alloc([128]) for _ in range(2)]
            mk = A.mark()
            Wk2, Wkb = A.alloc([8, 128], BF16)
            Wkd, Wkdb = A.alloc([8, 2, 128], BF16)
            Wksd, Wksdb = A.alloc([8, 2, 128], BF16)
            Wv, Wvb = A.alloc([8, 128], BF16)
            dma(POOL, Wq2, win_v[l][:, :, 0:512], "wa", [], [Wqb])
            dma(POOL, Wk2, win_v[l][:, :, 512:640], "wa", [], [Wkb])
            dma(POOL, Wv, win_v[l][:, :, 640:768], "wa", [], [Wvb])
            dma(POOL, woa, wout_v[l][:, 0:4, :], "wa", [], [woab])
            cp(Wqs2, Wq2, [Wqb], [Wqsb])
            q4 = Wq2.rearrange("p k (h e) -> p k h e", h=8)
            qs4 = Wqs2.rearrange("p k (h e) -> p k h e", h=8)
            cp(qs4[:, :, :, 0:8], q4[:, :, :, 8:16], [Wqb], [Wqsb])
            cp(qs4[:, :, :, 8:16], q4[:, :, :, 0:8], [Wqb], [Wqsb])
            for g in range(2):
                for hf in range(2):
                    cp(Wkd[:, :, g, hf * 64:(hf + 1) * 64], Wk2[:, :, g * 64:(g + 1) * 64], [Wkb], [Wkdb])
                    cp(Wksd[:, :, g, hf * 64:(hf + 1) * 64], Wk2[:, :, g * 64:(g + 1) * 64], [Wkb], [Wksdb])
                    cp(Wksd[:, :, g, hf * 64:hf * 64 + 8], Wk2[:, :, g * 64 + 8:g * 64 + 16], [Wkb], [Wksdb])
                    cp(Wksd[:, :, g, hf * 64 + 8:hf * 64 + 16], Wk2[:, :, g * 64:g * 64 + 8], [Wkb], [Wksdb])
            dma(SP, esink, sink_d[l], "small", [], [esinkb])
            dma(SP, anb, an_d[l], "small", [], [anbb])
            act(esink, esink, AF.Exp, [esinkb], [esinkb])
            P.op(DVE, lambda e: e.memset(Va[:, :, :, 64:66], 1.0), [], [Vab])
            for g in range(2):
                for par in range(2):
                    P.op(DVE, lambda e, t_=kz[g][par][0]: e.memset(t_, 0.0), [], [kz[g][par][1]])
            rcn = 0
            for g in range(2 if lim >= 2 else 0):
                for q in range(NQ):
                    qs = slice(q * 512, (q + 1) * 512)
                    bk, bkb = nb()
                    for k in range(8):
                        mm(bk[:, :], Wkd[:, k, g, :], hT[:, k, qs], [Wkdb, hb[q]], [bkb], start=(k == 0), stop=(k == 7))
                    b2, b2b = nb()
                    for k in range(8):
                        mm(b2[:, :], Wksd[:, k, g, :], hT[:, k, qs], [Wksdb, hb[q]], [b2b], start=(k == 0), stop=(k == 7))
                    for par in range(2):
                        pr = slice(par * 64, par * 64 + 64)
                        rr_ = slice(par * 64, par * 64 + 16)
                        kzt, kzb = kz[g][par]
                        act(kzt[pr, qs], bk[pr, :], AF.Copy, [bkb], [kzb])
                        a1, a1b = r1[rcn % 2]
                        a2, a2b = r2[rcn % 2]
                        rcn += 1
                        for ii in range(4):
                            sl = slice(q * 512 + ii * 128, q * 512 + (ii + 1) * 128)
                            if ii > 0:
                                a1, a1b = r1[rcn % 2]
                                a2, a2b = r2[rcn % 2]
                                rcn += 1
                            tt(a1[rr_], bk[rr_, ii * 128:(ii + 1) * 128], ropeC[rr_, sl], ALU.mult, [bkb, ropeb], [a1b])
                            tt(a2[rr_], b2[rr_, ii * 128:(ii + 1) * 128], ropeS[rr_, sl], ALU.mult, [b2b, ropeb], [a2b])
                            tt(kzt[rr_, sl], a1[rr_], a2[rr_], ALU.add, [a1b, a2b], [kzb])
            for q in range(NQ if lim >= 3 else 0):
                bk, bkb = nb()
                for ii in range(4):
                    tsl = slice(q * 512 + ii * 128, q * 512 + (ii + 1) * 128)
                    for k in range(8):
                        mm(bk[:, ii * 128:(ii + 1) * 128], hT[:, k, tsl], Wv[:, k, :], [Wvb, hb[q]], [bkb], start=(k == 0), stop=(k == 7))
                cp(Va[:, q * 4:(q + 1) * 4, :, 0:64], bk[:, :].rearrange("p (i g e) -> p i g e", i=4, g=2), [bkb], [Vab])
            A.release(mk)
            qT = [A.alloc([4, 128], BF16) for _ in range(2)]
            PT = [A.alloc([4, 128], BF16) for _ in range(6)]
            of32 = [A.alloc([512]) for _ in range(2)]
            onb2 = [A.alloc([512], BF16) for _ in range(2)]
            aTt = [A.alloc([4, 128], BF16) for _ in range(2)]
            sm = [A.alloc([24]) for _ in range(2)]
            junk2, junk2b = A.alloc([512])
            ptc = 0
            for n in range(NT if lim >= 4 else 0):
                q = n // 4
                nsl = slice(n * 128, (n + 1) * 128)
                qt, qtb = qT[n % 2]
                bk, bkb = nb()
                b2, b2b = nb()
                for c4 in range(4):
                    for k in range(8):
                        mm(bk[:, c4 * 128:(c4 + 1) * 128], Wq2[:, k, c4 * 128:(c4 + 1) * 128], hT[:, k, nsl], [Wqb, hb[q]], [bkb], start=(k == 0), stop=(k == 7))
                    for k in range(8):
                        mm(b2[:, c4 * 128:(c4 + 1) * 128], Wqs2[:, k, c4 * 128:(c4 + 1) * 128], hT[:, k, nsl], [Wqsb, hb[q]], [b2b], start=(k == 0), stop=(k == 7))
                act(qt, bk[:, :].rearrange("p (c t) -> p c t", c=4), AF.Copy, [bkb], [qtb])
                for c4 in range(4):
                    for par in range(2):
                        rr_ = slice(par * 64, par * 64 + 16)
                        a1, a1b = r1[rcn % 2]
                        a2, a2b = r2[rcn % 2]
                        rcn += 1
                        tt(a1[rr_], bk[rr_, c4 * 128:(c4 + 1) * 128], ropeC[rr_, nsl], ALU.mult, [bkb, ropeb], [a1b])
                        tt(a2[rr_], b2[rr_, c4 * 128:(c4 + 1) * 128], ropeS[rr_, nsl], ALU.mult, [b2b, ropeb], [a2b])
                        tt(qt[rr_, c4, :], a1[rr_], a2[rr_], ALU.add, [a1b, a2b], [qtb])
                of_, ofb = of32[n % 2]
                sm_, smb = sm[n % 2]
                if lim < 5:
                    continue
                for g in range(2):
                    js = [j for j in (n - 1, n, n + 1) if 0 <= j < NT]
                    pts = []
                    for j in js:
                        sb_, sbb = nb()
                        for par in range(2):
                            mm(sb_[:, par * 256:(par + 1) * 256], kz[g][par][0][:, j * 128:(j + 1) * 128], qt[:, 2 * g:2 * g + 2, :],
                               [kz[g][par][1], qtb], [sbb])
                        pt, ptb = PT[ptc % 6]
                        ptc += 1
                        act(pt, sb_[:, :].rearrange("p (h c) -> p h c", h=4), AF.Exp, [sbb], [ptb], scale=0.125)
                        if j != n:
                            mi = 0 if j == n - 1 else 1
                            tt(pt, pt, K["amask"][:, mi, :, :], ALU.mult, [ptb, K["amask_b"]], [ptb])
                        pts.append((pt, ptb))
                    if lim < 6:
                        continue
                    ob, obb = nb()
                    for s4 in range(4):
                        for ji, j in enumerate(js):
                            mm(ob[:, s4 * 66:(s4 + 1) * 66], pts[ji][0][:, s4, :], Va[:, j, g, :], [pts[ji][1], Vab], [obb],
                               start=(ji == 0), stop=(ji == len(js) - 1), skip=True)
                    obv = ob[:, 0:264].rearrange("p (h e) -> p h e", h=4)
                    tt(sm_[:, g * 4:(g + 1) * 4].rearrange("p (par cc) -> p par cc", par=2),
                       obv[:, :, 64].rearrange("p (par cc) -> p par cc", par=2),
                       esink[:, g * 4:(g + 1) * 4].rearrange("p (cc par) -> p par cc", par=2), ALU.add, [obb, esinkb], [smb])
                    recip(sm_[:, 8 + g * 4: 8 + (g + 1) * 4], sm_[:, g * 4:(g + 1) * 4], [smb], [smb])
                    for s4 in range(4):
                        h = 4 * g + 2 * (s4 % 2) + (s4 // 2)
                        ts(of_[:, h * 64:(h + 1) * 64], obv[:, s4, 0:64], sm_[:, 8 + g * 4 + s4: 9 + g * 4 + s4], None, ALU.mult, None, [obb, smb], [ofb])
                if lim < 7:
                    continue
                act(junk2, of_, AF.Square, [ofb], [junk2b, smb], accum=sm_[:, 16:17])
                rsqrt_chain(sm_[:, 16:17], sm_[:, 16:17], 1.0 / 512, [smb], [smb], None)
                on_, onb_ = onb2[n % 2]
                stt(on_, of_, sm_[:, 16:17], anb, ALU.mult, ALU.mult, [ofb, smb, anbb], [onb_])
                bk, bkb = nb()
                bv = bk.bitcast(BF16)
                for c4 in range(4):
                    tr(bv[:, c4 * 128:(c4 + 1) * 128], on_[:, c4 * 128:(c4 + 1) * 128], ident_bf, [onb_] + CB, [bkb])
                at_, atb = aTt[n % 2]
                cp(at_, bv[:, 0:512].rearrange("p (c t) -> p c t", c=4), [bkb], [atb])
                for hf in range(2):
                    bk, bkb = nb()
                    for c4 in range(4):
                        mm(bk[:, :], at_[:, c4, :], woa[:, c4, hf * 512:(hf + 1) * 512], [atb, woab], [bkb], start=(c4 == 0), stop=(c4 == 3))
                    x_update(n, hf, bk[:, :], bkb, 0)
            A.release(m)
            if stop == "mix":
                continue

            m = A.mark()
            G, Gb = A.alloc([NT, 128])
            P.op(DVE, lambda e: e.memset(G, 0.0), [], [Gb])
            wr, wrb = A.alloc([8, E])
            brb, brbb = A.alloc([E])
            bgu, bgub = A.alloc([E, 16])
            m2 = A.mark()
            GT, GTb = A.alloc([S])
            bdn, bdnb = A.alloc([D])
            dma(SP, wr, wr_d[l].rearrange("(k p) e -> p k e", p=128), "small", [], [wrb], ncok=True)
            dma(SP, brb, br_d[l], "small", [], [brbb])
            dma(SP, bgu, bgu_d[l], "small", [], [bgub])
            P.op(DVE, lambda e: e.memset(bdn, 0.0), [], [bdnb])
            dma(SP, bdn[0:E], bd_d[l], "small", [], [bdnb])
            ts(bgu[:, :, 8:16], bgu[:, :, 8:16], 1.0, None, ALU.add, None, [bgub], [bgub])
            rt = [A.alloc([3, E]) for _ in range(2)]
            rs_ = [A.alloc([16]) for _ in range(2)]

            def router(q, hn, hnb):
                for ii in range(4):
                    i = q * 4 + ii
                    lg3, lgb = rt[i % 2]
                    s_, s_b = rs_[i % 2]
                    bk, bkb = nb()
                    for k in range(8):
                        mm(bk[:, 0:E], hn[:, k, ii * 128:(ii + 1) * 128], wr[:, k, :], [hnb, wrb], [bkb], start=(k == 0), stop=(k == 7))
                    tt(lg3[:, 0, :], bk[:, 0:E], brb, ALU.add, [bkb, brbb], [lgb])
                    P.op(DVE, lambda e, o=s_[:, 0:8], a=lg3[:, 0, :]: e.max(out=o, in_=a), [lgb], [s_b])
                    ts(s_[:, 8:9], s_[:, 0:1], -1.0, None, ALU.mult, None, [s_b], [s_b])
                    act(lg3[:, 1, :], lg3[:, 0, :], AF.Exp, [lgb, s_b], [lgb], bias=s_[:, 8:9])
                    stt(lg3[:, 2, :], lg3[:, 0, :], s_[:, 3:4], lg3[:, 1, :], ALU.is_ge, ALU.mult, [lgb, s_b], [lgb, s_b], accum=s_[:, 9:10])
                    recip(s_[:, 10:11], s_[:, 9:10], [s_b], [s_b])
                    ts(G[:, i, 0:E], lg3[:, 2, :], s_[:, 10:11], None, ALU.mult, None, [lgb, s_b], [Gb])
                    bk, bkb = nb()
                    tr(bk[:, 0:128], G[:, i, :], ident_f, [Gb] + CB, [bkb])
                    cp(GT[:, i * 128:(i + 1) * 128], bk[:, 0:128], [bkb], [GTb])

            norm_phase(l, 1, router=router)
            for i in range(NT):
                for hf in range(2):
                    bk, bkb = nb()
                    mm(bk[:, :], GT[:, i * 128:(i + 1) * 128], bdn[:, hf * 512:(hf + 1) * 512], [GTb, bdnb], [bkb])
                    x_update(i, hf, bk[:, :], bkb, 1)

            A.release(m2)
            Wg = [A.alloc([8, 2, 512], BF16) for _ in range(2)]
            Wd = [A.alloc([4, D], BF16) for _ in range(2)]
            actT = [A.alloc([4, 512], BF16) for _ in range(2)]
            Tm = [[A.alloc([512]) for _ in range(4)] for _ in range(1)]
            u = 0
            ac = 0
            for e_ in range(E):
                wgv = wgu_d[l, e_].rearrange("(k p) (j n) -> p k j n", p=128, j=2)
                for f2 in range(2):
                    wg, wgb = Wg[u % 2]
                    wd, wdb = Wd[u % 2]
                    u += 1
                    for j_ in range(2):
                        dma(POOL, wg[:, :, j_, :], wgv[:, :, j_, f2 * 512:(f2 + 1) * 512], ("wg", u % 2), [], [wgb])
                    dma(POOL, wd, wd_d[l, e_, f2 * 512:(f2 + 1) * 512, :].rearrange("(c p) n -> p c n", p=128), ("wd", u % 2), [], [wdb])
                    for q in range(NQ):
                        qs = slice(q * 512, (q + 1) * 512)
                        at_, atb = actT[ac % 2]
                        tm = Tm[0]
                        ac += 1
                        for fc4 in range(4):
                            fc = f2 * 4 + fc4
                            pg, pgb = nb()
                            for k in range(8):
                                mm(pg[:, :], wg[:, k, 0, fc4 * 128:(fc4 + 1) * 128], hT[:, k, qs], [wgb, hb[q]], [pgb], start=(k == 0), stop=(k == 7))
                            pl, plb = nb()
                            for k in range(8):
                                mm(pl[:, :], wg[:, k, 1, fc4 * 128:(fc4 + 1) * 128], hT[:, k, qs], [wgb, hb[q]], [plb], start=(k == 0), stop=(k == 7))
                            ts(tm[0][0], pg[:, :], bgu[:, e_, fc:fc + 1], 7.0, ALU.add, ALU.min, [pgb, bgub], [tm[0][1]])
                            act(tm[1][0], tm[0][0], AF.Sigmoid, [tm[0][1]], [tm[1][1]], scale=1.702)
                            ts(tm[2][0], pl[:, :], bgu[:, e_, 8 + fc:9 + fc], 8.0, ALU.add, ALU.min, [plb, bgub], [tm[2][1]])
                            tt(tm[3][0], tm[0][0], tm[1][0], ALU.mult, [tm[0][1], tm[1][1]], [tm[3][1]])
                            stt(at_[:, fc4, :], tm[2][0], -6.0, tm[3][0], ALU.max, ALU.mult, [tm[2][1], tm[3][1]], [atb])
                        for ii in range(4):
                            i = q * 4 + ii
                            for hf in range(2):
                                pd, pdb = nb()
                                for fc4 in range(4):
                                    mm(pd[:, :], at_[:, fc4, ii * 128:(ii + 1) * 128], wd[:, fc4, hf * 512:(hf + 1) * 512], [atb, wdb], [pdb],
                                       start=(fc4 == 0), stop=(fc4 == 3))
                                x_update(i, hf, pd[:, :], pdb, 1, gate=G[:, i, e_:e_ + 1], gate_b=Gb)
            A.release(m)

        m = A.mark()
        ss, ssb = A.alloc([NT])
        junk, junkb = A.alloc([D])
        yo = [A.alloc([D]) for _ in range(2)]
        fnb, fnbb = A.alloc([D])
        dma(SP, fnb, fn_d, "small", [], [fnbb])
        for i in range(NT):
            act(junk, X[:, i, :], AF.Square, [xb[i]], [junkb, ssb], accum=ss[:, i:i + 1])
        rsqrt_chain(ss, ss, 1.0 / D, [ssb], [ssb], None)
        yv = y_d.rearrange("(i p) d -> i p d", p=128)
        finals = []
        for i in range(NT):
            yo_, yob = yo[i % 2]
            stt(yo_, X[:, i, :], ss[:, i:i + 1], fnb, ALU.mult, ALU.mult, [xb[i], ssb, fnbb], [yob])
            finals.append(P.dma(SP, lambda e, o=yv[i], a=yo_: e.dma_start(out=o, in_=a), ("y", i % 2), [yob], []))
        A.release(m)
        P.emit(final_waits=finals)
        build.info = dict(n_ins=len(P.ins), n_sems=P.n_sems, sbuf_peak_words=A.peak)
    return nc


def make_in_maps(inp, S, E, L):
    B = inp["x"].shape[0]
    f = lambda a: np.ascontiguousarray(np.asarray(a))
    cst = _consts()
    shared = {
        "w_ada": f(inp["w_ada"]), "b_ada": f(np.asarray(inp["b_ada"]).reshape(L, 1, 6 * D)),
        "norm1T": f(np.asarray(inp["norm1"]).reshape(L, 8, 128).transpose(0, 2, 1)),
        "norm2T": f(np.asarray(inp["norm2"]).reshape(L, 8, 128).transpose(0, 2, 1)),
        "w_in": f(inp["w_in"]),
        "sinkb": f(np.broadcast_to(np.asarray(inp["attn_sink"])[:, None, :], (L, 128, 8))),
        "anormb": f(np.broadcast_to(np.asarray(inp["attn_norm"])[:, None, :], (L, 128, 512))),
        "lblT": f(np.asarray(inp["hg_lb_logits"]).reshape(L, 2, 4, 128).transpose(3, 0, 1, 2).reshape(128, L, 8)),
        "hgnT": f(np.asarray(inp["hg_norm"]).T),
        "w_out": f(inp["w_out"]), "w_router": f(inp["w_router"]),
        "brb": f(np.broadcast_to(np.asarray(inp["b_router"])[:, None, :], (L, 128, E))),
        "w_gu": f(inp["w_gu"]),
        "bguT": f(np.asarray(inp["b_gu"]).reshape(L, E, 16, 128).transpose(0, 3, 1, 2)),
        "w_down": f(inp["w_down"]), "b_down": f(inp["b_down"]),
        "fnb": f(np.broadcast_to(np.asarray(inp["final_norm"])[None, :], (128, D))),
    }
    for n, _, _ in CONST_SPECS:
        shared["c_" + n] = cst[n]
    maps = []
    for b in range(B):
        m = dict(shared)
        m["x"] = f(inp["x"][b])
        m["cT"] = f(np.asarray(inp["c"][b]).reshape(8, 128).T)
        m["pos16"] = f(np.broadcast_to(np.asarray(inp["positions"][b]).astype(np.int32)[None, :], (128, S)))
        maps.append(m)
    return maps


def kernel(**inputs):
    B, S, _ = inputs["x"].shape
    L = inputs["w_ada"].shape[0]
    E = inputs["w_router"].shape[2]
    nc = build(S, E, L)
    maps = make_in_maps(inputs, S, E, L)
    res = run_bass_kernel_spmd(nc, maps, core_ids=list(range(B)))
    return np.stack([np.asarray(r["y"]) for r in res.results], axis=0).astype(np.float32)
```

```python
import contextlib
import numpy as np
import ml_dtypes
import concourse.bass as bass
import concourse.mybir as mybir
from concourse.bass_utils import run_bass_kernel_spmd

F32 = mybir.dt.float32
BF16 = mybir.dt.bfloat16
I32 = mybir.dt.int32
AF = mybir.ActivationFunctionType
ALU = mybir.AluOpType

D = 1024
NIN = 3328
EPS = 1e-6
PE, ACT, DVE, POOL, SP = "pe", "act", "dve", "pool", "sp"
ENGS = [PE, ACT, DVE, POOL, SP]
SEM_CAP = 30000


class Buf:
    __slots__ = ("w", "r", "excl")

    def __init__(self, excl=False):
        self.w = None
        self.r = []
        self.excl = excl


class Ins:
    __slots__ = ("id", "eng", "fn", "deps", "is_dma", "dma_sem", "dma_val", "signal", "sig_sem", "sig_val")


class Prog:
    def __init__(self, nc):
        self.nc = nc
        self.ins = []
        self.q = {e: [] for e in ENGS}
        self.dma_cnt = {}
        self.last_dma = {}
        self.fence_ids = set()
        self.fence_epoch = 0
        self.eng_epoch = {e: 0 for e in ENGS}

    def fence(self):
        ids = set(self.last_dma.values())
        for e in ENGS:
            if self.q[e]:
                ids.add(self.q[e][-1].id)
        self.fence_ids = ids
        self.fence_epoch += 1

    def _rec(self, eng, fn, reads, writes):
        i = Ins()
        i.id = len(self.ins)
        i.eng = eng
        i.fn = fn
        i.is_dma = False
        i.signal = False
        i.dma_sem = None
        deps = set()
        if self.eng_epoch[eng] != self.fence_epoch:
            deps.update(self.fence_ids)
            self.eng_epoch[eng] = self.fence_epoch
        for b in reads:
            if b.w is not None:
                deps.add(b.w)
            if b.excl:
                deps.update(r for r in b.r if self.ins[r].eng != eng)
        for b in writes:
            if b.w is not None:
                deps.add(b.w)
            deps.update(b.r)
        if eng == PE:
            deps = {d for d in deps if self.ins[d].eng != PE}
        i.deps = deps
        for b in reads:
            b.r.append(i.id)
        for b in writes:
            b.w = i.id
            b.r = []
        self.ins.append(i)
        self.q[eng].append(i)
        return i.id

    def op(self, eng, fn, reads=(), writes=()):
        return self._rec(eng, fn, list(reads), list(writes))

    def dma(self, eng, fn, slot, reads=(), writes=()):
        iid = self._rec(eng, fn, list(reads), list(writes))
        i = self.ins[iid]
        if slot in self.last_dma:
            i.deps.add(self.last_dma[slot])
        self.last_dma[slot] = iid
        i.is_dma = True
        self.dma_cnt[slot] = self.dma_cnt.get(slot, 0) + 16
        i.dma_sem = slot
        i.dma_val = self.dma_cnt[slot]
        return iid

    def emit(self, final_waits=()):
        nc = self.nc
        ins = self.ins
        for i in ins:
            for d in i.deps:
                if not ins[d].is_dma:
                    ins[d].signal = True
        nsig = {e: 0 for e in ENGS}
        for e in ENGS:
            for i in self.q[e]:
                if i.signal and not i.is_dma:
                    n = nsig[e]
                    i.sig_sem = (e, n // SEM_CAP)
                    i.sig_val = n % SEM_CAP + 1
                    nsig[e] = n + 1
        keys = set()
        for i in ins:
            if i.is_dma:
                keys.add(("dma", i.dma_sem))
            elif i.signal:
                keys.add(i.sig_sem)
        keys = sorted(keys, key=str)
        self.n_sems = len(keys)
        with contextlib.ExitStack() as st:
            sems = {}
            for k in keys:
                sems[k] = st.enter_context(nc.semaphore("s%d" % len(sems)))
            block = st.enter_context(nc.Block())
            engobj = {PE: "tensor", ACT: "scalar", DVE: "vector", POOL: "gpsimd", SP: "sync"}

            def make(e):
                def body(eng):
                    known = {}
                    for i in self.q[e]:
                        need = {}
                        for d in i.deps:
                            di = ins[d]
                            if di.is_dma:
                                k, v = ("dma", di.dma_sem), di.dma_val
                            else:
                                k, v = di.sig_sem, di.sig_val
                            if v > need.get(k, 0):
                                need[k] = v
                        for k, v in need.items():
                            if known.get(k, 0) < v:
                                eng.wait_ge(sems[k], v)
                                known[k] = v
                        r = i.fn(eng)
                        if i.is_dma:
                            r.then_inc(sems[("dma", i.dma_sem)], 16)
                        elif i.signal:
                            r.then_inc(sems[i.sig_sem], 1)
                    if e == SP:
                        for d in final_waits:
                            di = ins[d]
                            eng.wait_ge(sems[("dma", di.dma_sem)], di.dma_val)
                return body

            for e in ENGS:
                if self.q[e] or e == SP:
                    getattr(block, engobj[e])(make(e))


class Arena:
    def __init__(self, t, nwords):
        self.t = t
        self.cap = nwords
        self.top = 0
        self.peak = 0
        self.prog = None

    def alloc(self, shape, dtype=F32):
        n = int(np.prod(shape))
        words = n if dtype in (F32, I32) else (n + 1) // 2
        words = (words + 15) // 16 * 16
        off = self.top
        self.top += words
        self.peak = max(self.peak, self.top)
        assert self.top <= self.cap, ("SBUF arena overflow", self.top, self.cap)
        ap = self.t[:, off:off + (n if dtype in (F32, I32) else (n + 1) // 2)]
        if dtype != F32:
            ap = ap.bitcast(dtype)
        if len(shape) == 2:
            ap = ap.rearrange("p (a b) -> p a b", a=shape[0])
        elif len(shape) == 3:
            ap = ap.rearrange("p (a b c) -> p a b c", a=shape[0], b=shape[1])
        elif len(shape) == 4:
            ap = ap.rearrange("p (a b c d) -> p a b c d", a=shape[0], b=shape[1], c=shape[2])
        return ap, Buf()

    def mark(self):
        return self.top

    def release(self, m):
        self.top = m
        if self.prog is not None:
            self.prog.fence()


def _consts():
    bf = ml_dtypes.bfloat16
    p = np.arange(128)
    c = {}
    c["ident_bf"] = np.eye(128, dtype=np.float32).astype(bf)
    c["ident_f"] = np.eye(128, dtype=np.float32)
    c["ones_bf"] = np.ones((128, 128), np.float32).astype(bf)
    c["ones_f"] = np.ones((128, 128), np.float32)
    same = (p[:, None] // 32) == (p[None, :] // 32)
    mf = (same & (p[:, None] <= p[None, :])).astype(np.float32)
    mb = (same & (p[:, None] >= p[None, :])).astype(np.float32)
    c["hmask"] = np.stack([np.tile(mf[:, None, :], (1, 4, 1)), np.tile(mb[:, None, :], (1, 4, 1))], 1).astype(bf)
    ml = (p[:, None] >= p[None, :]).astype(np.float32)
    mr = (p[:, None] <= p[None, :]).astype(np.float32)
    c["amask"] = np.stack([np.tile(ml[:, None, :], (1, 4, 1)), np.tile(mr[:, None, :], (1, 4, 1))], 1).astype(bf)
    c["bm4"] = (p[:, None] // 32 == np.arange(4)[None, :]).astype(np.float32)
    c["rmask"] = np.tile((np.arange(512) % 32 != 0).astype(np.float32)[None, :], (128, 1))
    invf = (500000.0 ** (-(np.arange(8, dtype=np.float32) * 2.0 / 16))).astype(np.float32)
    rp = np.zeros((128, 2), np.float32)
    for base in (0, 64):
        rp[base:base + 16, 0] = np.concatenate([invf, invf])
        rp[base:base + 16, 1] = np.concatenate([-np.ones(8), np.ones(8)])
    c["ropec"] = rp
    return c


CONST_SPECS = [("ident_bf", [128, 128], BF16), ("ident_f", [128, 128], F32), ("ones_bf", [128, 128], BF16),
               ("ones_f", [128, 128], F32), ("hmask", [128, 2, 4, 128], BF16), ("amask", [128, 2, 4, 128], BF16),
               ("bm4", [128, 4], F32), ("rmask", [128, 512], F32), ("ropec", [128, 2], F32)]


def build(S, E, L, stop=None):
    NT = S // 128
    NQ = S // 512
    NCH = S // 32
    nc = bass.Bass("TRN2", target_bir_lowering=False)

    def din(name, shape, dt=F32):
        return nc.dram_tensor(name, list(shape), dt, kind="ExternalInput").ap()

    x_d = din("x", [S, D])
    cT_d = din("cT", [128, 8])
    pos_d = din("pos16", [128, S], I32)
    wada_d = din("w_ada", [L, D, 6 * D])
    bada_d = din("b_ada", [L, 1, 6 * D])
    n1_d = din("norm1T", [L, 128, 8])
    n2_d = din("norm2T", [L, 128, 8])
    win_d = din("w_in", [L, D, NIN])
    sink_d = din("sinkb", [L, 128, 8])
    an_d = din("anormb", [L, 128, 512])
    lbl_d = din("lblT", [128, L, 8])
    hgn_d = din("hgnT", [128, L])
    wout_d = din("w_out", [L, D, D])
    wr_d = din("w_router", [L, D, E])
    br_d = din("brb", [L, 128, E])
    wgu_d = din("w_gu", [L, E, D, 2 * D])
    bgu_d = din("bguT", [L, 128, E, 16])
    wd_d = din("w_down", [L, E, D, D])
    bd_d = din("b_down", [L, E, D])
    fn_d = din("fnb", [128, D])
    cd = {n: din("c_" + n, sh, dt) for n, sh, dt in CONST_SPECS}
    y_d = nc.dram_tensor("y", [S, D], F32, kind="ExternalOutput").ap()

    st = contextlib.ExitStack()
    with st:
        NW = 53200
        arena_t = st.enter_context(nc.sbuf_tensor("arena", [128, NW], F32))
        A = Arena(arena_t, NW)
        banks = []
        for i in range(8):
            t = st.enter_context(nc.psum_tensor("pb%d" % i, [128, 512], F32))
            banks.append((t, Buf(excl=True)))
        bctr = [0]

        def nb():
            b = banks[bctr[0] % 8]
            bctr[0] += 1
            return b

        P = Prog(nc)
        A.prog = P

        def mm(out, lhsT, rhs, R, W, start=True, stop=True, skip=False):
            P.op(PE, lambda e: e.matmul(out, lhsT=lhsT, rhs=rhs, start=start, stop=stop, skip_group_check=skip), R, W)

        def tr(out, in_, ident, R, W):
            P.op(PE, lambda e: e.transpose(out, in_, ident), R, W)

        def act(out, in_, func, R, W, scale=1.0, bias=None, accum=None):
            def f(e):
                kw = {}
                if bias is not None:
                    kw["bias"] = bias
                if accum is not None:
                    kw["accum_out"] = accum
                return e.activation(out=out, in_=in_, func=func, scale=scale, **kw)
            P.op(ACT, f, R, W)

        def ts(out, in0, s1, s2, op0, op1, R, W):
            if s2 is None:
                P.op(DVE, lambda e: e.tensor_scalar(out=out, in0=in0, scalar1=s1, scalar2=None, op0=op0), R, W)
            else:
                P.op(DVE, lambda e: e.tensor_scalar(out=out, in0=in0, scalar1=s1, scalar2=s2, op0=op0, op1=op1), R, W)

        def tt(out, in0, in1, op, R, W):
            P.op(DVE, lambda e: e.tensor_tensor(out=out, in0=in0, in1=in1, op=op), R, W)

        def stt(out, in0, sc, in1, op0, op1, R, W, accum=None):
            if accum is None:
                P.op(DVE, lambda e: e.scalar_tensor_tensor(out=out, in0=in0, scalar=sc, in1=in1, op0=op0, op1=op1), R, W)
            else:
                P.op(DVE, lambda e: e.scalar_tensor_tensor(out=out, in0=in0, scalar=sc, in1=in1, op0=op0, op1=op1, accum_out=accum), R, W)

        def cp(out, in_, R, W):
            P.op(DVE, lambda e: e.tensor_copy(out=out, in_=in_), R, W)

        def recip(out, in_, R, W):
            P.op(DVE, lambda e: e.reciprocal(out=out, in_=in_), R, W)

        def dma(eng, out, in_, slot, R, W, ncok=False):
            if ncok:
                P.dma(eng, lambda e: e.dma_start(out=out, in_=in_, allow_slow_non_contiguous=True), slot, R, W)
            else:
                P.dma(eng, lambda e: e.dma_start(out=out, in_=in_), slot, R, W)

        def rsqrt_chain(dst, src, scale, R_, W_, tmpb):
            ts(dst, src, scale, EPS, ALU.mult, ALU.add, R_, W_)
            act(dst, dst, AF.Sqrt, W_, W_)
            recip(dst, dst, W_, W_)

        X, _ = A.alloc([NT, D])
        xb = [Buf() for _ in range(NT)]
        K = {}
        for n, sh, dt in CONST_SPECS:
            K[n], kb_ = A.alloc(sh[1:], dt)
            K[n + "_b"] = kb_
        for n, sh, dt in CONST_SPECS:
            np_ = sh[0]
            dma(SP, K[n][0:np_], cd[n], "const", [], [K[n + "_b"]])
        ident_bf, ident_f, ones_bf, ones_f = K["ident_bf"], K["ident_f"], K["ones_bf"], K["ones_f"]
        CB = [K[n + "_b"] for n, _, _ in CONST_SPECS]
        condT, condb = A.alloc([8])
        lbT, lbb = A.alloc([L, 8])
        omlT, _ = A.alloc([L, 8])
        lbm1T, _ = A.alloc([L, 8])
        hgnT, hgnb = A.alloc([L])
        gbc, gbcb = A.alloc([2, D])
        modT, modTb = A.alloc([4, 8])
        hT, _ = A.alloc([8, S], BF16)
        hb = [Buf() for _ in range(NQ)]
        ropeC, ropeb = A.alloc([S])
        ropeS, _ = A.alloc([S])

        dma(SP, X, x_d.rearrange("(i p) d -> p i d", p=128), "x", [], xb)
        dma(SP, condT, cT_d, "small", [], [condb])
        dma(SP, hgnT, hgn_d, "small", [], [hgnb])

        m0 = A.mark()
        t8, t8b = A.alloc([8])
        act(t8, condT, AF.Sigmoid, [condb], [t8b])
        tt(condT, condT, t8, ALU.mult, [condb, t8b], [condb])
        lraw, lrawb = A.alloc([L, 8])
        lsum, lsumb = A.alloc([8])
        dma(SP, lraw, lbl_d, "small", [], [lrawb])
        act(lraw, lraw, AF.Exp, [lrawb], [lrawb])
        cp(lsum, lraw[:, 0, :], [lrawb], [lsumb])
        for l in range(1, L):
            tt(lsum, lsum, lraw[:, l, :], ALU.add, [lsumb, lrawb], [lsumb])
        recip(lsum, lsum, [lsumb], [lsumb])
        P.op(DVE, lambda e: e.memset(lbT[:, 0, :], 0.0), [], [lbb])
        for l in range(1, L):
            tt(t8, lraw[:, l, :], lsum, ALU.mult, [lrawb, lsumb], [t8b])
            tt(lbT[:, l, :], lbT[:, l - 1, :], t8, ALU.add, [lbb, t8b], [lbb])
        ts(omlT, lbT, -1.0, 1.0, ALU.mult, ALU.add, [lbb], [lbb])
        ts(lbm1T, lbT, -1.0, None, ALU.add, None, [lbb], [lbb])
        TWO_PI = float(2 * np.pi)
        posi, posb = A.alloc([S], I32)
        ang, angb = A.alloc([S])
        rr, rrb = A.alloc([S])
        ni, nib = A.alloc([S], I32)
        dma(SP, posi, pos_d, "small", [], [posb])
        rc = K["ropec"]
        cp(ang, posi, [posb], [angb])
        ts(ang, ang, rc[:, 0:1], None, ALU.mult, None, [angb, K["ropec_b"]], [angb])
        ts(rr, ang, 1.0 / TWO_PI, None, ALU.mult, None, [angb], [rrb])
        cp(ni, rr, [rrb], [nib])
        cp(rr, ni, [nib], [rrb])
        stt(ang, rr, -TWO_PI, ang, ALU.mult, ALU.add, [rrb, angb], [angb])

        def wrap_sin(dst, shift, dstb):
            ts(rr, ang, shift, None, ALU.add, None, [angb], [rrb])
            ts(ni.bitcast(F32), rr, float(np.pi), -TWO_PI, ALU.is_gt, ALU.mult, [rrb], [nib])
            tt(rr, rr, ni.bitcast(F32), ALU.add, [rrb, nib], [rrb])
            ts(ni.bitcast(F32), rr, float(-np.pi), TWO_PI, ALU.is_lt, ALU.mult, [rrb], [nib])
            tt(rr, rr, ni.bitcast(F32), ALU.add, [rrb, nib], [rrb])
            act(dst, rr, AF.Sin, [rrb], [dstb])

        wrap_sin(ropeC, float(np.pi / 2), ropeb)
        wrap_sin(ropeS, 0.0, ropeb)
        ts(ropeS, ropeS, rc[:, 1:2], None, ALU.mult, None, [ropeb, K["ropec_b"]], [ropeb])
        A.release(m0)

        def norm_phase(l, which, router=None):
            m = A.mark()
            ss, ssb = A.alloc([NT])
            junk, junkb = A.alloc([D])
            xn_dt = F32 if router is not None else BF16
            xn = [A.alloc([D], xn_dt) for _ in range(4)]
            for i in range(NT):
                act(junk, X[:, i, :], AF.Square, [xb[i]], [junkb, ssb], accum=ss[:, i:i + 1])
            rsqrt_chain(ss, ss, 1.0 / D, [ssb], [ssb], None)
            aT = modT[:, 2 * which, :]
            bT = modT[:, 2 * which + 1, :]
            if router is not None:
                hn, hnb = A.alloc([8, 512])
            for q in range(NQ):
                for ii in range(4):
                    i = q * 4 + ii
                    ts(xn[ii][0], X[:, i, :], ss[:, i:i + 1], None, ALU.mult, None, [xb[i], ssb], [xn[ii][1]])
                for k in range(8):
                    bk, bkb = nb()
                    if router is None:
                        bv = bk.bitcast(BF16)
                        for ii in range(4):
                            tr(bv[:, ii * 128:(ii + 1) * 128], xn[ii][0][:, k * 128:(k + 1) * 128], ident_bf, [xn[ii][1]] + CB, [bkb])
                        ts(hT[:, k, q * 512:(q + 1) * 512], bv[:, 0:512], aT[:, k:k + 1], bT[:, k:k + 1], ALU.mult, ALU.add,
                           [bkb, modTb], [hb[q]])
                    else:
                        for ii in range(4):
                            tr(bk[:, ii * 128:(ii + 1) * 128], xn[ii][0][:, k * 128:(k + 1) * 128], ident_f, [xn[ii][1]] + CB, [bkb])
                        ts(hn[:, k, :], bk[:, 0:512], aT[:, k:k + 1], bT[:, k:k + 1], ALU.mult, ALU.add, [bkb, modTb], [hnb])
                        act(hT[:, k, q * 512:(q + 1) * 512], hn[:, k, :], AF.Copy, [hnb], [hb[q]])
                if router is not None:
                    router(q, hn, hnb)
            A.release(m)

        def x_update(i, half, psum, psb, gidx, gate=None, gate_b=None):
            tmp, tmpb = xtmp[0]
            xtc[0] += 1
            sl = slice(half * 512, (half + 1) * 512)
            tt(tmp, psum, gbc[:, gidx, sl], ALU.mult, [psb, gbcb], [tmpb])
            if gate is None:
                tt(X[:, i, sl], X[:, i, sl], tmp, ALU.add, [xb[i], tmpb], [xb[i]])
            else:
                stt(X[:, i, sl], tmp, gate, X[:, i, sl], ALU.mult, ALU.add, [tmpb, gate_b, xb[i]], [xb[i]])

        xtmp = [A.alloc([512]) for _ in range(1)]
        xtc = [0]
        win_v = [win_d[l].rearrange("(k p) n -> p k n", p=128) for l in range(L)]
        wout_v = [wout_d[l].rearrange("(k p) n -> p k n", p=128) for l in range(L)]

        for l in range(L if stop != "pro" else 0):
            m = A.mark()
            modrow, modrowb = A.alloc([6 * D])
            badar = [A.alloc([512]) for _ in range(2)]
            wblk = [A.alloc([8, 512]) for _ in range(2)]
            n12, n12b = A.alloc([2, 8])
            dma(SP, n12[:, 0, :], n1_d[l], "small", [], [n12b])
            dma(SP, n12[:, 1, :], n2_d[l], "small", [], [n12b])
            wav = wada_d[l].rearrange("(k p) n -> p k n", p=128)
            for j in range(12):
                wb_, wbb = wblk[j % 2]
                dma(SP, wb_, wav[:, :, j * 512:(j + 1) * 512], ("wada", j % 2), [], [wbb])
                bd_, bdb_ = badar[j % 2]
                dma(SP, bd_[0:1], bada_d[l][:, j * 512:(j + 1) * 512], ("bada", j % 2), [], [bdb_])
                bk, bkb = nb()
                for k in range(8):
                    mm(bk[0:1, :], condT[:, k:k + 1], wb_[:, k, :], [condb, wbb], [bkb], start=(k == 0), stop=(k == 7))
                tt(modrow[0:1, j * 512:(j + 1) * 512], bk[0:1, :], bd_[0:1, :], ALU.add,
                   [bkb, bdb_], [modrowb])
            for gi, v in enumerate((2, 5)):
                for hf in range(2):
                    bk, bkb = nb()
                    mm(bk[:, :], ones_f[0:1, 0:128], modrow[0:1, v * D + hf * 512: v * D + (hf + 1) * 512], [modrowb] + CB, [bkb])
                    cp(gbc[:, gi, hf * 512:(hf + 1) * 512], bk[:, :], [bkb], [gbcb])
            bk, bkb = nb()
            for vi, v in enumerate((1, 0, 4, 3)):
                for k in range(8):
                    col = (vi * 8 + k) * 2
                    mm(bk[:, col:col + 2], modrow[0:1, v * D + k * 128: v * D + (k + 1) * 128], ones_f[0:1, 0:2], [modrowb] + CB, [bkb])
            bkv = bk[:, 0:64].rearrange("p (v k two) -> p v k two", v=4, k=8)
            cp(modT, bkv[:, :, :, 0], [bkb], [modTb])
            for w_ in range(2):
                ts(modT[:, 2 * w_, :], modT[:, 2 * w_, :], 1.0, None, ALU.add, None, [modTb], [modTb])
                tt(modT[:, 2 * w_, :], modT[:, 2 * w_, :], n12[:, w_, :], ALU.mult, [modTb, n12b], [modTb])
            A.release(m)

            if stop == "ada":
                continue
            norm_phase(l, 0)
            if stop == "n1":
                continue

            for hh in range(4):
                m = A.mark()
                Wh, Whb = A.alloc([8, 5, 128], BF16)
                woh, wohb = A.alloc([D], BF16)
                for j, c0 in enumerate((768, 1280, 1792, 2304, 2816)):
                    dma(POOL, Wh[:, :, j, :], win_v[l][:, :, c0 + hh * 128: c0 + (hh + 1) * 128], "wh", [], [Whb])
                dma(POOL, woh, wout_d[l][512 + hh * 128: 512 + (hh + 1) * 128, :], "wh", [], [wohb])
                qside = [A.alloc([S], BF16) for _ in range(2)]
                kside = [A.alloc([S], BF16) for _ in range(2)]
                ktok = [A.alloc([NT, 128], BF16) for _ in range(2)]
                vtok, vtokb = A.alloc([NT, 128], BF16)
                Gt = [A.alloc([NCH]) for _ in range(2)]
                oacc, oaccb = A.alloc([S])
                T = [A.alloc([512]) for _ in range(8)]
                for q in range(NQ):
                    qs = slice(q * 512, (q + 1) * 512)
                    bk, bkb = nb()
                    for k in range(8):
                        mm(bk[:, :], Wh[:, k, 0, :], hT[:, k, qs], [Whb, hb[q]], [bkb], start=(k == 0), stop=(k == 7))
                    act(T[0][0], bk[:, :], AF.Sigmoid, [bkb], [T[0][1]])
                    tt(T[1][0], bk[:, :], T[0][0], ALU.mult, [bkb, T[0][1]], [T[1][1]])
                    for d in range(2):
                        li = d * 4 + hh
                        bk, bkb = nb()
                        for k in range(8):
                            mm(bk[:, :], Wh[:, k, 1 + d, :], hT[:, k, qs], [Whb, hb[q]], [bkb], start=(k == 0), stop=(k == 7))
                        act(T[2][0], bk[:, :], AF.Sigmoid, [bkb], [T[2][1]])
                        ts(T[3][0], T[2][0], omlT[:, l, li:li + 1], lbT[:, l, li:li + 1], ALU.mult, ALU.add, [T[2][1], lbb], [T[3][1]])
                        ts(T[4][0], T[2][0], lbm1T[:, l, li:li + 1], omlT[:, l, li:li + 1], ALU.mult, ALU.add, [T[2][1], lbb], [T[4][1]])
                        act(T[3][0], T[3][0], AF.Ln, [T[3][1]], [T[3][1]])
                        P.op(DVE, lambda e, o=T[5][0], a=K["rmask"], b_=T[3][0]: e.tensor_tensor_scan(
                            out=o, data0=a, data1=b_, initial=0.0, op0=ALU.mult, op1=ALU.add), [T[3][1], K["rmask_b"]], [T[5][1]])
                        bend = T[5][0].rearrange("p (c j) -> p c j", j=32)[:, :, 31]
                        if d == 0:
                            act(T[6][0], T[5][0], AF.Exp, [T[5][1]], [T[6][1]])
                            act(T[7][0], T[5][0], AF.Exp, [T[5][1]], [T[7][1]], scale=-1.0)
                        else:
                            tt(T[3][0], T[5][0], T[3][0], ALU.subtract, [T[5][1], T[3][1]], [T[3][1]])
                            act(T[6][0], T[3][0], AF.Exp, [T[3][1]], [T[6][1]], scale=-1.0)
                            act(T[7][0], T[3][0], AF.Exp, [T[3][1]], [T[7][1]])
                        act(Gt[d][0][:, q * 16:(q + 1) * 16], bend, AF.Exp, [T[5][1]], [Gt[d][1]])
                        tt(qside[d][0][:, qs], T[1][0], T[6][0], ALU.mult, [T[1][1], T[6][1]], [qside[d][1]])
                        tt(kside[d][0][:, qs], T[4][0], T[7][0], ALU.mult, [T[4][1], T[7][1]], [kside[d][1]])
                        bk, bkb = nb()
                        bv = bk.bitcast(BF16)
                        for ii in range(4):
                            tr(bv[:, ii * 128:(ii + 1) * 128], kside[d][0][:, q * 512 + ii * 128: q * 512 + (ii + 1) * 128], ident_bf,
                               [kside[d][1]] + CB, [bkb])
                        cp(ktok[d][0][:, q * 4:(q + 1) * 4, :], bv[:, 0:512].rearrange("p (i c) -> p i c", i=4), [bkb], [ktok[d][1]])
                    bk, bkb = nb()
                    for ii in range(4):
                        tsl = slice(q * 512 + ii * 128, q * 512 + (ii + 1) * 128)
                        for k in range(8):
                            mm(bk[:, ii * 128:(ii + 1) * 128], hT[:, k, tsl], Wh[:, k, 3, :], [Whb, hb[q]], [bkb], start=(k == 0), stop=(k == 7))
                    cp(vtok[:, q * 4:(q + 1) * 4, :], bk[:, :].rearrange("p (i c) -> p i c", i=4), [bkb], [vtokb])
                Wst = [A.alloc([128]) for _ in range(2)]
                Ub = [A.alloc([16, 128], BF16) for _ in range(1)]
                Vm = [A.alloc([4, 4, 128], BF16) for _ in range(1)]
                Abd = [A.alloc([4, 128], BF16) for _ in range(1)]
                onT = [A.alloc([512], BF16) for _ in range(1)]
                wcnt = 0
                have_prev = False
                for d in range(2):
                    have_prev = False
                    qorder = range(NQ) if d == 0 else range(NQ - 1, -1, -1)
                    for qi_, q in enumerate(qorder):
                        qs = slice(q * 512, (q + 1) * 512)
                        vm, vmb = Vm[0]
                        ub, ubb = Ub[0]
                        abd, abdb = Abd[0]
                        for ii in range(4):
                            for r in range(4):
                                ts(vm[:, ii, r, :], vtok[:, q * 4 + ii, :], K["bm4"][:, r:r + 1], None, ALU.mult, None,
                                   [vtokb, K["bm4_b"]], [vmb])
                        zb = []
                        for ii in range(4):
                            bk, bkb = nb()
                            mm(bk[:, :], ktok[d][0][:, q * 4 + ii, :], vm[:, ii, :, :], [ktok[d][1], vmb], [bkb])
                            zb.append((bk, bkb))
                        corder = range(16) if d == 0 else range(15, -1, -1)
                        uvalid = [False] * 16
                        for cl in corder:
                            c = q * 16 + cl
                            zc = zb[cl // 4][0][:, (cl % 4) * 128:(cl % 4 + 1) * 128]
                            zcb = zb[cl // 4][1]
                            wnew, wnewb = Wst[wcnt % 2]
                            wold, woldb = Wst[(wcnt + 1) % 2]
                            if not have_prev:
                                cp(wnew, zc, [zcb], [wnewb])
                            else:
                                gidx = (c - 1) if d == 0 else c
                                gsc = Gt[d][0][:, gidx:gidx + 1]
                                act(ub[:, cl, :], wold, AF.Copy, [woldb, Gt[d][1]], [ubb], scale=gsc)
                                uvalid[cl] = True
                                stt(wnew, wold, gsc, zc, ALU.mult, ALU.add, [woldb, Gt[d][1], zcb], [wnewb])
                            have_prev = True
                            wcnt += 1
                        bk, bkb = nb()
                        for ii in range(4):
                            tsl = slice(q * 512 + ii * 128, q * 512 + (ii + 1) * 128)
                            mm(bk[:, ii * 128:(ii + 1) * 128], kside[d][0][:, tsl], qside[d][0][:, tsl], [kside[d][1], qside[d][1]], [bkb])
                        tt(abd, bk[:, :].rearrange("p (i c) -> p i c", i=4), K["hmask"][:, d, :, :], ALU.mult, [bkb, K["hmask_b"]], [abdb])
                        ob, obb = nb()
                        first = True
                        for ii in range(4):
                            mm(ob[:, ii * 128:(ii + 1) * 128], vtok[:, q * 4 + ii, :], abd[:, ii, :], [vtokb, abdb], [obb],
                               start=first, stop=False, skip=True)
                            first = False
                        nl = sum(uvalid)
                        cnt = 0
                        for cl in range(16):
                            if not uvalid[cl]:
                                continue
                            cnt += 1
                            mm(ob[:, cl * 32:(cl + 1) * 32], ub[:, cl, :], qside[d][0][:, q * 512 + cl * 32: q * 512 + (cl + 1) * 32],
                               [ubb, qside[d][1]], [obb], start=False, stop=(cnt == nl), skip=True)
                        if d == 0:
                            cp(oacc[:, qs], ob[:, :], [obb], [oaccb])
                        else:
                            o_, o_b = T[0]
                            tt(o_, ob[:, :], oacc[:, qs], ALU.add, [obb, oaccb], [o_b])
                            act(T[1][0].bitcast(BF16)[:, 0:512], o_, AF.Square, [o_b], [T[1][1]])
                            bk, bkb = nb()
                            mm(bk[:, :], ones_bf, T[1][0].bitcast(BF16)[:, 0:512], [T[1][1]] + CB, [bkb])
                            ts(T[2][0], bk[:, :], 1.0 / 128, EPS, ALU.mult, ALU.add, [bkb], [T[2][1]])
                            act(T[2][0], T[2][0], AF.Sqrt, [T[2][1]], [T[2][1]])
                            recip(T[2][0], T[2][0], [T[2][1]], [T[2][1]])
                            stt(T[3][0], o_, hgnT[:, l:l + 1], T[2][0], ALU.mult, ALU.mult, [o_b, hgnb, T[2][1]], [T[3][1]])
                            bk, bkb = nb()
                            for k in range(8):
                                mm(bk[:, :], Wh[:, k, 4, :], hT[:, k, qs], [Whb, hb[q]], [bkb], start=(k == 0), stop=(k == 7))
                            act(T[4][0], bk[:, :], AF.Sigmoid, [bkb], [T[4][1]])
                            tt(T[4][0], bk[:, :], T[4][0], ALU.mult, [bkb, T[4][1]], [T[4][1]])
                            on_, onb_ = onT[0]
                            tt(on_, T[3][0], T[4][0], ALU.mult, [T[3][1], T[4][1]], [onb_])
                            for ii in range(4):
                                for hf in range(2):
                                    bk, bkb = nb()
                                    mm(bk[:, :], on_[:, ii * 128:(ii + 1) * 128], woh[:, hf * 512:(hf + 1) * 512], [onb_, wohb], [bkb])
                                    x_update(q * 4 + ii, hf, bk[:, :], bkb, 0)
                A.release(m)

            if stop == "hg":
                continue
            m = A.mark()
            lim = int(stop[2:]) if (stop or "").startswith("at") else 99
            Wq2, Wqb = A.alloc([8, 512], BF16)
            Wqs2, Wqsb = A.alloc([8, 512], BF16)
            woa, woab = A.alloc([4, D], BF16)
            kz = [[A.alloc([S], BF16) for _ in range(2)] for _ in range(2)]
            Va, Vab = A.alloc([NT, 2, 66], BF16)
            esink, esinkb = A.alloc([8])
            anb, anbb = A.alloc([512])
            r1 = [A.alloc([128]) for _ in range(2)]
            r2 = [A.alloc([128]) for _ in range(2)]
            mk = A.mark()
            Wk2, Wkb = A.alloc([8, 128], BF16)
            Wkd, Wkdb = A.alloc([8, 2, 128], BF16)
            Wksd, Wksdb = A.alloc([8, 2, 128], BF16)
            Wv, Wvb = A.alloc([8, 128], BF16)
            dma(POOL, Wq2, win_v[l][:, :, 0:512], "wa", [], [Wqb])
            dma(POOL, Wk2, win_v[l][:, :, 512:640], "wa", [], [Wkb])
            dma(POOL, Wv, win_v[l][:, :, 640:768], "wa", [], [Wvb])
            dma(POOL, woa, wout_v[l][:, 0:4, :], "wa", [], [woab])
            cp(Wqs2, Wq2, [Wqb], [Wqsb])
            q4 = Wq2.rearrange("p k (h e) -> p k h e", h=8)
            qs4 = Wqs2.rearrange("p k (h e) -> p k h e", h=8)
            cp(qs4[:, :, :, 0:8], q4[:, :, :, 8:16], [Wqb], [Wqsb])
            cp(qs4[:, :, :, 8:16], q4[:, :, :, 0:8], [Wqb], [Wqsb])
            for g in range(2):
                for hf in range(2):
                    cp(Wkd[:, :, g, hf * 64:(hf + 1) * 64], Wk2[:, :, g * 64:(g + 1) * 64], [Wkb], [Wkdb])
                    cp(Wksd[:, :, g, hf * 64:(hf + 1) * 64], Wk2[:, :, g * 64:(g + 1) * 64], [Wkb], [Wksdb])
                    cp(Wksd[:, :, g, hf * 64:hf * 64 + 8], Wk2[:, :, g * 64 + 8:g * 64 + 16], [Wkb], [Wksdb])
                    cp(Wksd[:, :, g, hf * 64 + 8:hf * 64 + 16], Wk2[:, :, g * 64:g * 64 + 8], [Wkb], [Wksdb])
            dma(SP, esink, sink_d[l], "small", [], [esinkb])
            dma(SP, anb, an_d[l], "small", [], [anbb])
            act(esink, esink, AF.Exp, [esinkb], [esinkb])
            P.op(DVE, lambda e: e.memset(Va[:, :, :, 64:66], 1.0), [], [Vab])
            for g in range(2):
                for par in range(2):
                    P.op(DVE, lambda e, t_=kz[g][par][0]: e.memset(t_, 0.0), [], [kz[g][par][1]])
            rcn = 0
            for g in range(2 if lim >= 2 else 0):
                for q in range(NQ):
                    qs = slice(q * 512, (q + 1) * 512)
                    bk, bkb = nb()
                    for k in range(8):
                        mm(bk[:, :], Wkd[:, k, g, :], hT[:, k, qs], [Wkdb, hb[q]], [bkb], start=(k == 0), stop=(k == 7))
                    b2, b2b = nb()
                    for k in range(8):
                        mm(b2[:, :], Wksd[:, k, g, :], hT[:, k, qs], [Wksdb, hb[q]], [b2b], start=(k == 0), stop=(k == 7))
                    for par in range(2):
                        pr = slice(par * 64, par * 64 + 64)
                        rr_ = slice(par * 64, par * 64 + 16)
                        kzt, kzb = kz[g][par]
                        act(kzt[pr, qs], bk[pr, :], AF.Copy, [bkb], [kzb])
                        a1, a1b = r1[rcn % 2]
                        a2, a2b = r2[rcn % 2]
                        rcn += 1
                        for ii in range(4):
                            sl = slice(q * 512 + ii * 128, q * 512 + (ii + 1) * 128)
                            if ii > 0:
                                a1, a1b = r1[rcn % 2]
                                a2, a2b = r2[rcn % 2]
                                rcn += 1
                            tt(a1[rr_], bk[rr_, ii * 128:(ii + 1) * 128], ropeC[rr_, sl], ALU.mult, [bkb, ropeb], [a1b])
                            tt(a2[rr_], b2[rr_, ii * 128:(ii + 1) * 128], ropeS[rr_, sl], ALU.mult, [b2b, ropeb], [a2b])
                            tt(kzt[rr_, sl], a1[rr_], a2[rr_], ALU.add, [a1b, a2b], [kzb])
            for q in range(NQ if lim >= 3 else 0):
                bk, bkb = nb()
                for ii in range(4):
                    tsl = slice(q * 512 + ii * 128, q * 512 + (ii + 1) * 128)
                    for k in range(8):
                        mm(bk[:, ii * 128:(ii + 1) * 128], hT[:, k, tsl], Wv[:, k, :], [Wvb, hb[q]], [bkb], start=(k == 0), stop=(k == 7))
                cp(Va[:, q * 4:(q + 1) * 4, :, 0:64], bk[:, :].rearrange("p (i g e) -> p i g e", i=4, g=2), [bkb], [Vab])
            A.release(mk)
            qT = [A.alloc([4, 128], BF16) for _ in range(2)]
            PT = [A.alloc([4, 128], BF16) for _ in range(6)]
            of32 = [A.alloc([512]) for _ in range(2)]
            onb2 = [A.alloc([512], BF16) for _ in range(2)]
            aTt = [A.alloc([4, 128], BF16) for _ in range(2)]
            sm = [A.alloc([24]) for _ in range(2)]
            junk2, junk2b = A.alloc([512])
            ptc = 0
            for n in range(NT if lim >= 4 else 0):
                q = n // 4
                nsl = slice(n * 128, (n + 1) * 128)
                qt, qtb = qT[n % 2]
                bk, bkb = nb()
                b2, b2b = nb()
                for c4 in range(4):
                    for k in range(8):
                        mm(bk[:, c4 * 128:(c4 + 1) * 128], Wq2[:, k, c4 * 128:(c4 + 1) * 128], hT[:, k, nsl], [Wqb, hb[q]], [bkb], start=(k == 0), stop=(k == 7))
                    for k in range(8):
                        mm(b2[:, c4 * 128:(c4 + 1) * 128], Wqs2[:, k, c4 * 128:(c4 + 1) * 128], hT[:, k, nsl], [Wqsb, hb[q]], [b2b], start=(k == 0), stop=(k == 7))
                act(qt, bk[:, :].rearrange("p (c t) -> p c t", c=4), AF.Copy, [bkb], [qtb])
                for c4 in range(4):
                    for par in range(2):
                        rr_ = slice(par * 64, par * 64 + 16)
                        a1, a1b = r1[rcn % 2]
                        a2, a2b = r2[rcn % 2]
                        rcn += 1
                        tt(a1[rr_], bk[rr_, c4 * 128:(c4 + 1) * 128], ropeC[rr_, nsl], ALU.mult, [bkb, ropeb], [a1b])
                        tt(a2[rr_], b2[rr_, c4 * 128:(c4 + 1) * 128], ropeS[rr_, nsl], ALU.mult, [b2b, ropeb], [a2b])
                        tt(qt[rr_, c4, :], a1[rr_], a2[rr_], ALU.add, [a1b, a2b], [qtb])
                of_, ofb = of32[n % 2]
                sm_, smb = sm[n % 2]
                if lim < 5:
                    continue
                for g in range(2):
                    js = [j for j in (n - 1, n, n + 1) if 0 <= j < NT]
                    pts = []
                    for j in js:
                        sb_, sbb = nb()
                        for par in range(2):
                            mm(sb_[:, par * 256:(par + 1) * 256], kz[g][par][0][:, j * 128:(j + 1) * 128], qt[:, 2 * g:2 * g + 2, :],
                               [kz[g][par][1], qtb], [sbb])
                        pt, ptb = PT[ptc % 6]
                        ptc += 1
                        act(pt, sb_[:, :].rearrange("p (h c) -> p h c", h=4), AF.Exp, [sbb], [ptb], scale=0.125)
                        if j != n:
                            mi = 0 if j == n - 1 else 1
                            tt(pt, pt, K["amask"][:, mi, :, :], ALU.mult, [ptb, K["amask_b"]], [ptb])
                        pts.append((pt, ptb))
                    if lim < 6:
                        continue
                    ob, obb = nb()
                    for s4 in range(4):
                        for ji, j in enumerate(js):
                            mm(ob[:, s4 * 66:(s4 + 1) * 66], pts[ji][0][:, s4, :], Va[:, j, g, :], [pts[ji][1], Vab], [obb],
                               start=(ji == 0), stop=(ji == len(js) - 1), skip=True)
                    obv = ob[:, 0:264].rearrange("p (h e) -> p h e", h=4)
                    tt(sm_[:, g * 4:(g + 1) * 4].rearrange("p (par cc) -> p par cc", par=2),
                       obv[:, :, 64].rearrange("p (par cc) -> p par cc", par=2),
                       esink[:, g * 4:(g + 1) * 4].rearrange("p (cc par) -> p par cc", par=2), ALU.add, [obb, esinkb], [smb])
                    recip(sm_[:, 8 + g * 4: 8 + (g + 1) * 4], sm_[:, g * 4:(g + 1) * 4], [smb], [smb])
                    for s4 in range(4):
                        h = 4 * g + 2 * (s4 % 2) + (s4 // 2)
                        ts(of_[:, h * 64:(h + 1) * 64], obv[:, s4, 0:64], sm_[:, 8 + g * 4 + s4: 9 + g * 4 + s4], None, ALU.mult, None, [obb, smb], [ofb])
                if lim < 7:
                    continue
                act(junk2, of_, AF.Square, [ofb], [junk2b, smb], accum=sm_[:, 16:17])
                rsqrt_chain(sm_[:, 16:17], sm_[:, 16:17], 1.0 / 512, [smb], [smb], None)
                on_, onb_ = onb2[n % 2]
                stt(on_, of_, sm_[:, 16:17], anb, ALU.mult, ALU.mult, [ofb, smb, anbb], [onb_])
                bk, bkb = nb()
                bv = bk.bitcast(BF16)
                for c4 in range(4):
                    tr(bv[:, c4 * 128:(c4 + 1) * 128], on_[:, c4 * 128:(c4 + 1) * 128], ident_bf, [onb_] + CB, [bkb])
                at_, atb = aTt[n % 2]
                cp(at_, bv[:, 0:512].rearrange("p (c t) -> p c t", c=4), [bkb], [atb])
                for hf in range(2):
                    bk, bkb = nb()
                    for c4 in range(4):
                        mm(bk[:, :], at_[:, c4, :], woa[:, c4, hf * 512:(hf + 1) * 512], [atb, woab], [bkb], start=(c4 == 0), stop=(c4 == 3))
                    x_update(n, hf, bk[:, :], bkb, 0)
            A.release(m)
            if stop == "mix":
                continue

            m = A.mark()
            G, Gb = A.alloc([NT, E])
            wr, wrb = A.alloc([8, E])
            brb, brbb = A.alloc([E])
            bgu, bgub = A.alloc([E, 16])
            m2 = A.mark()
            GT, GTb = A.alloc([S])
            bdn, bdnb = A.alloc([D])
            dma(SP, wr, wr_d[l].rearrange("(k p) e -> p k e", p=128), "small", [], [wrb], ncok=True)
            dma(SP, brb, br_d[l], "small", [], [brbb])
            dma(SP, bgu, bgu_d[l], "small", [], [bgub])
            P.op(DVE, lambda e: e.memset(bdn, 0.0), [], [bdnb])
            dma(SP, bdn[0:E], bd_d[l], "small", [], [bdnb])
            ts(bgu[:, :, 8:16], bgu[:, :, 8:16], 1.0, None, ALU.add, None, [bgub], [bgub])
            Gp, Gpb = A.alloc([128])
            P.op(DVE, lambda e: e.memset(Gp, 0.0), [], [Gpb])
            rt = [A.alloc([3, E]) for _ in range(2)]
            rs_ = [A.alloc([16]) for _ in range(2)]

            def router(q, hn, hnb):
                for ii in range(4):
                    i = q * 4 + ii
                    lg3, lgb = rt[i % 2]
                    s_, s_b = rs_[i % 2]
                    bk, bkb = nb()
                    for k in range(8):
                        mm(bk[:, 0:E], hn[:, k, ii * 128:(ii + 1) * 128], wr[:, k, :], [hnb, wrb], [bkb], start=(k == 0), stop=(k == 7))
                    tt(lg3[:, 0, :], bk[:, 0:E], brb, ALU.add, [bkb, brbb], [lgb])
                    P.op(DVE, lambda e, o=s_[:, 0:8], a=lg3[:, 0, :]: e.max(out=o, in_=a), [lgb], [s_b])
                    ts(s_[:, 8:9], s_[:, 0:1], -1.0, None, ALU.mult, None, [s_b], [s_b])
                    act(lg3[:, 1, :], lg3[:, 0, :], AF.Exp, [lgb, s_b], [lgb], bias=s_[:, 8:9])
                    stt(lg3[:, 2, :], lg3[:, 0, :], s_[:, 3:4], lg3[:, 1, :], ALU.is_ge, ALU.mult, [lgb, s_b], [lgb, s_b], accum=s_[:, 9:10])
                    recip(s_[:, 10:11], s_[:, 9:10], [s_b], [s_b])
                    ts(G[:, i, :], lg3[:, 2, :], s_[:, 10:11], None, ALU.mult, None, [lgb, s_b], [Gb])
                    cp(Gp[:, 0:E], G[:, i, :], [Gb], [Gpb])
                    bk, bkb = nb()
                    tr(bk[:, 0:128], Gp, ident_f, [Gpb] + CB, [bkb])
                    cp(GT[:, i * 128:(i + 1) * 128], bk[:, 0:128], [bkb], [GTb])

            norm_phase(l, 1, router=router)
            for i in range(NT):
                for hf in range(2):
                    bk, bkb = nb()
                    mm(bk[:, :], GT[:, i * 128:(i + 1) * 128], bdn[:, hf * 512:(hf + 1) * 512], [GTb, bdnb], [bkb])
                    x_update(i, hf, bk[:, :], bkb, 1)

            A.release(m2)
            Wg = [A.alloc([8, 2, 512], BF16) for _ in range(2)]
            Wd = [A.alloc([4, D], BF16) for _ in range(2)]
            actT = [A.alloc([4, 512], BF16) for _ in range(2)]
            Tm = [[A.alloc([512]), A.alloc([512], BF16), A.alloc([512]), A.alloc([512], BF16)] for _ in range(2)]
            fcn = 0
            ac = 0
            units = [(e_, f2) for e_ in range(E) for f2 in range(2)]

            def load_unit(ui):
                e_, f2 = units[ui]
                wg, wgb = Wg[ui % 2]
                wd, wdb = Wd[ui % 2]
                wgv = wgu_d[l, e_].rearrange("(k p) (j n) -> p k j n", p=128, j=2)
                for j_ in range(2):
                    dma(POOL, wg[:, :, j_, :], wgv[:, :, j_, f2 * 512:(f2 + 1) * 512], ("wg", ui % 2), [], [wgb])
                dma(POOL, wd, wd_d[l, e_, f2 * 512:(f2 + 1) * 512, :].rearrange("(c p) n -> p c n", p=128), ("wd", ui % 2), [], [wdb])

            load_unit(0)
            for ui in range(len(units)):
                e_, f2 = units[ui]
                if ui + 1 < len(units):
                    load_unit(ui + 1)
                wg, wgb = Wg[ui % 2]
                wd, wdb = Wd[ui % 2]
                for c_ in range(4):
                    tt(wd[:, c_, :], wd[:, c_, :], gbc[:, 1, :], ALU.mult, [wdb, gbcb], [wdb])
                if True:
                    for q in range(NQ):
                        qs = slice(q * 512, (q + 1) * 512)
                        at_, atb = actT[ac % 2]
                        ac += 1
                        for fc4 in range(4):
                            fc = f2 * 4 + fc4
                            tm = Tm[fcn % 2]
                            fcn += 1
                            pg, pgb = nb()
                            for k in range(8):
                                mm(pg[:, :], wg[:, k, 0, fc4 * 128:(fc4 + 1) * 128], hT[:, k, qs], [wgb, hb[q]], [pgb], start=(k == 0), stop=(k == 7))
                            pl, plb = nb()
                            for k in range(8):
                                mm(pl[:, :], wg[:, k, 1, fc4 * 128:(fc4 + 1) * 128], hT[:, k, qs], [wgb, hb[q]], [plb], start=(k == 0), stop=(k == 7))
                            ts(tm[0][0], pg[:, :], bgu[:, e_, fc:fc + 1], 7.0, ALU.add, ALU.min, [pgb, bgub], [tm[0][1]])
                            act(tm[1][0], tm[0][0], AF.Sigmoid, [tm[0][1]], [tm[1][1]], scale=1.702)
                            ts(tm[2][0], pl[:, :], bgu[:, e_, 8 + fc:9 + fc], 8.0, ALU.add, ALU.min, [plb, bgub], [tm[2][1]])
                            P.op(POOL, lambda e, o=tm[3][0], a=tm[0][0], b_=tm[1][0]: e.tensor_tensor(out=o, in0=a, in1=b_, op=ALU.mult),
                                 [tm[0][1], tm[1][1]], [tm[3][1]])
                            stt(at_[:, fc4, :], tm[2][0], -6.0, tm[3][0], ALU.max, ALU.mult, [tm[2][1], tm[3][1]], [atb])
                        for ii in range(4):
                            i = q * 4 + ii
                            for hf in range(2):
                                pd, pdb = nb()
                                for fc4 in range(4):
                                    mm(pd[:, :], at_[:, fc4, ii * 128:(ii + 1) * 128], wd[:, fc4, hf * 512:(hf + 1) * 512], [atb, wdb], [pdb],
                                       start=(fc4 == 0), stop=(fc4 == 3))
                                sl_ = slice(hf * 512, (hf + 1) * 512)
                                stt(X[:, i, sl_], pd[:, :], G[:, i, e_:e_ + 1], X[:, i, sl_], ALU.mult, ALU.add, [pdb, Gb, xb[i]], [xb[i]])
            A.release(m)

        m = A.mark()
        ss, ssb = A.alloc([NT])
        junk, junkb = A.alloc([D])
        yo = [A.alloc([D]) for _ in range(2)]
        fnb, fnbb = A.alloc([D])
        dma(SP, fnb, fn_d, "small", [], [fnbb])
        for i in range(NT):
            act(junk, X[:, i, :], AF.Square, [xb[i]], [junkb, ssb], accum=ss[:, i:i + 1])
        rsqrt_chain(ss, ss, 1.0 / D, [ssb], [ssb], None)
        yv = y_d.rearrange("(i p) d -> i p d", p=128)
        finals = []
        for i in range(NT):
            yo_, yob = yo[i % 2]
            stt(yo_, X[:, i, :], ss[:, i:i + 1], fnb, ALU.mult, ALU.mult, [xb[i], ssb, fnbb], [yob])
            finals.append(P.dma(SP, lambda e, o=yv[i], a=yo_: e.dma_start(out=o, in_=a), ("y", i % 2), [yob], []))
        A.release(m)
        P.emit(final_waits=finals)
        build.info = dict(n_ins=len(P.ins), n_sems=P.n_sems, sbuf_peak_words=A.peak)
    return nc


def make_in_maps(inp, S, E, L):
    B = inp["x"].shape[0]
    f = lambda a: np.ascontiguousarray(np.asarray(a))
    cst = _consts()
    shared = {
        "w_ada": f(inp["w_ada"]), "b_ada": f(np.asarray(inp["b_ada"]).reshape(L, 1, 6 * D)),
        "norm1T": f(np.asarray(inp["norm1"]).reshape(L, 8, 128).transpose(0, 2, 1)),
        "norm2T": f(np.asarray(inp["norm2"]).reshape(L, 8, 128).transpose(0, 2, 1)),
        "w_in": f(inp["w_in"]),
        "sinkb": f(np.broadcast_to(np.asarray(inp["attn_sink"])[:, None, :], (L, 128, 8))),
        "anormb": f(np.broadcast_to(np.asarray(inp["attn_norm"])[:, None, :], (L, 128, 512))),
        "lblT": f(np.asarray(inp["hg_lb_logits"]).reshape(L, 2, 4, 128).transpose(3, 0, 1, 2).reshape(128, L, 8)),
        "hgnT": f(np.asarray(inp["hg_norm"]).T),
        "w_out": f(inp["w_out"]), "w_router": f(inp["w_router"]),
        "brb": f(np.broadcast_to(np.asarray(inp["b_router"])[:, None, :], (L, 128, E))),
        "w_gu": f(inp["w_gu"]),
        "bguT": f(np.asarray(inp["b_gu"]).reshape(L, E, 16, 128).transpose(0, 3, 1, 2)),
        "w_down": f(inp["w_down"]), "b_down": f(inp["b_down"]),
        "fnb": f(np.broadcast_to(np.asarray(inp["final_norm"])[None, :], (128, D))),
    }
    for n, _, _ in CONST_SPECS:
        shared["c_" + n] = cst[n]
    maps = []
    for b in range(B):
        m = dict(shared)
        m["x"] = f(inp["x"][b])
        m["cT"] = f(np.asarray(inp["c"][b]).reshape(8, 128).T)
        m["pos16"] = f(np.broadcast_to(np.asarray(inp["positions"][b]).astype(np.int32)[None, :], (128, S)))
        maps.append(m)
    return maps


def kernel(**inputs):
    B, S, _ = inputs["x"].shape
    L = inputs["w_ada"].shape[0]
    E = inputs["w_router"].shape[2]
    nc = build(S, E, L)
    maps = make_in_maps(inputs, S, E, L)
    res = run_bass_kernel_spmd(nc, maps, core_ids=list(range(B)))
    return np.stack([np.asarray(r["y"]) for r in res.results], axis=0).astype(np.float32)
```

```python
import contextlib
import numpy as np
import ml_dtypes
import concourse.bass as bass
import concourse.mybir as mybir
from concourse.bass_utils import run_bass_kernel_spmd

F32 = mybir.dt.float32
BF16 = mybir.dt.bfloat16
I32 = mybir.dt.int32
AF = mybir.ActivationFunctionType
ALU = mybir.AluOpType

D = 1024
NIN = 3328
EPS = 1e-6
PE, ACT, DVE, POOL, SP = "pe", "act", "dve", "pool", "sp"
ENGS = [PE, ACT, DVE, POOL, SP]
SEM_CAP = 30000


class Buf:
    __slots__ = ("w", "r", "excl")

    def __init__(self, excl=False):
        self.w = None
        self.r = []
        self.excl = excl


class Ins:
    __slots__ = ("id", "eng", "fn", "deps", "is_dma", "dma_sem", "dma_val", "signal", "sig_sem", "sig_val")


class Prog:
    def __init__(self, nc):
        self.nc = nc
        self.ins = []
        self.q = {e: [] for e in ENGS}
        self.dma_cnt = {}
        self.last_dma = {}
        self.fence_ids = set()
        self.fence_epoch = 0
        self.eng_epoch = {e: 0 for e in ENGS}

    def fence(self):
        ids = set(self.last_dma.values())
        for e in ENGS:
            if self.q[e]:
                ids.add(self.q[e][-1].id)
        self.fence_ids = ids
        self.fence_epoch += 1

    def _rec(self, eng, fn, reads, writes):
        i = Ins()
        i.id = len(self.ins)
        i.eng = eng
        i.fn = fn
        i.is_dma = False
        i.signal = False
        i.dma_sem = None
        deps = set()
        if self.eng_epoch[eng] != self.fence_epoch:
            deps.update(self.fence_ids)
            self.eng_epoch[eng] = self.fence_epoch
        for b in reads:
            if b.w is not None:
                deps.add(b.w)
            if b.excl:
                deps.update(r for r in b.r if self.ins[r].eng != eng)
        for b in writes:
            if b.w is not None:
                deps.add(b.w)
            deps.update(b.r)
        if eng == PE:
            deps = {d for d in deps if self.ins[d].eng != PE}
        i.deps = deps
        for b in reads:
            b.r.append(i.id)
        for b in writes:
            b.w = i.id
            b.r = []
        self.ins.append(i)
        self.q[eng].append(i)
        return i.id

    def op(self, eng, fn, reads=(), writes=()):
        return self._rec(eng, fn, list(reads), list(writes))

    def dma(self, eng, fn, slot, reads=(), writes=()):
        iid = self._rec(eng, fn, list(reads), list(writes))
        i = self.ins[iid]
        if slot in self.last_dma:
            i.deps.add(self.last_dma[slot])
        self.last_dma[slot] = iid
        i.is_dma = True
        self.dma_cnt[slot] = self.dma_cnt.get(slot, 0) + 16
        i.dma_sem = slot
        i.dma_val = self.dma_cnt[slot]
        return iid

    def emit(self, final_waits=()):
        nc = self.nc
        ins = self.ins
        for i in ins:
            for d in i.deps:
                if not ins[d].is_dma:
                    ins[d].signal = True
        nsig = {e: 0 for e in ENGS}
        for e in ENGS:
            for i in self.q[e]:
                if i.signal and not i.is_dma:
                    n = nsig[e]
                    i.sig_sem = (e, n // SEM_CAP)
                    i.sig_val = n % SEM_CAP + 1
                    nsig[e] = n + 1
        keys = set()
        for i in ins:
            if i.is_dma:
                keys.add(("dma", i.dma_sem))
            elif i.signal:
                keys.add(i.sig_sem)
        keys = sorted(keys, key=str)
        self.n_sems = len(keys)
        with contextlib.ExitStack() as st:
            sems = {}
            for k in keys:
                sems[k] = st.enter_context(nc.semaphore("s%d" % len(sems)))
            block = st.enter_context(nc.Block())
            engobj = {PE: "tensor", ACT: "scalar", DVE: "vector", POOL: "gpsimd", SP: "sync"}

            def make(e):
                def body(eng):
                    known = {}
                    for i in self.q[e]:
                        need = {}
                        for d in i.deps:
                            di = ins[d]
                            if di.is_dma:
                                k, v = ("dma", di.dma_sem), di.dma_val
                            else:
                                k, v = di.sig_sem, di.sig_val
                            if v > need.get(k, 0):
                                need[k] = v
                        for k, v in need.items():
                            if known.get(k, 0) < v:
                                eng.wait_ge(sems[k], v)
                                known[k] = v
                        r = i.fn(eng)
                        if i.is_dma:
                            r.then_inc(sems[("dma", i.dma_sem)], 16)
                        elif i.signal:
                            r.then_inc(sems[i.sig_sem], 1)
                    if e == SP:
                        for d in final_waits:
                            di = ins[d]
                            eng.wait_ge(sems[("dma", di.dma_sem)], di.dma_val)
                return body

            for e in ENGS:
                if self.q[e] or e == SP:
                    getattr(block, engobj[e])(make(e))


class Arena:
    def __init__(self, t, nwords):
        self.t = t
        self.cap = nwords
        self.top = 0
        self.peak = 0
        self.prog = None

    def alloc(self, shape, dtype=F32):
        n = int(np.prod(shape))
        words = n if dtype in (F32, I32) else (n + 1) // 2
        words = (words + 15) // 16 * 16
        off = self.top
        self.top += words
        self.peak = max(self.peak, self.top)
        assert self.top <= self.cap, ("SBUF arena overflow", self.top, self.cap)
        ap = self.t[:, off:off + (n if dtype in (F32, I32) else (n + 1) // 2)]
        if dtype != F32:
            ap = ap.bitcast(dtype)
        if len(shape) == 2:
            ap = ap.rearrange("p (a b) -> p a b", a=shape[0])
        elif len(shape) == 3:
            ap = ap.rearrange("p (a b c) -> p a b c", a=shape[0], b=shape[1])
        elif len(shape) == 4:
            ap = ap.rearrange("p (a b c d) -> p a b c d", a=shape[0], b=shape[1], c=shape[2])
        return ap, Buf()

    def mark(self):
        return self.top

    def release(self, m):
        self.top = m
        if self.prog is not None:
            self.prog.fence()


def _consts():
    bf = ml_dtypes.bfloat16
    p = np.arange(128)
    c = {}
    c["ident_bf"] = np.eye(128, dtype=np.float32).astype(bf)
    c["ident_f"] = np.eye(128, dtype=np.float32)
    c["ones_bf"] = np.ones((128, 128), np.float32).astype(bf)
    c["ones_f"] = np.ones((128, 128), np.float32)
    same = (p[:, None] // 32) == (p[None, :] // 32)
    mf = (same & (p[:, None] <= p[None, :])).astype(np.float32)
    mb = (same & (p[:, None] >= p[None, :])).astype(np.float32)
    c["hmask"] = np.stack([np.tile(mf[:, None, :], (1, 4, 1)), np.tile(mb[:, None, :], (1, 4, 1))], 1).astype(bf)
    ml = (p[:, None] >= p[None, :]).astype(np.float32)
    mr = (p[:, None] <= p[None, :]).astype(np.float32)
    c["amask"] = np.stack([np.tile(ml[:, None, :], (1, 4, 1)), np.tile(mr[:, None, :], (1, 4, 1))], 1).astype(bf)
    c["bm4"] = (p[:, None] // 32 == np.arange(4)[None, :]).astype(np.float32)
    c["rmask"] = np.tile((np.arange(512) % 32 != 0).astype(np.float32)[None, :], (128, 1))
    invf = (500000.0 ** (-(np.arange(8, dtype=np.float32) * 2.0 / 16))).astype(np.float32)
    rp = np.zeros((128, 2), np.float32)
    for base in (0, 64):
        rp[base:base + 16, 0] = np.concatenate([invf, invf])
        rp[base:base + 16, 1] = np.concatenate([-np.ones(8), np.ones(8)])
    c["ropec"] = rp
    return c


CONST_SPECS = [("ident_bf", [128, 128], BF16), ("ident_f", [128, 128], F32), ("ones_bf", [128, 128], BF16),
               ("ones_f", [128, 128], F32), ("hmask", [128, 2, 4, 128], BF16), ("amask", [128, 2, 4, 128], BF16),
               ("bm4", [128, 4], F32), ("rmask", [128, 512], F32), ("ropec", [128, 2], F32)]


def build(S, E, L, stop=None):
    NT = S // 128
    NQ = S // 512
    NCH = S // 32
    nc = bass.Bass("TRN2", target_bir_lowering=False)

    def din(name, shape, dt=F32):
        return nc.dram_tensor(name, list(shape), dt, kind="ExternalInput").ap()

    x_d = din("x", [S, D])
    cT_d = din("cT", [128, 8])
    pos_d = din("pos16", [128, S], I32)
    wada_d = din("w_ada", [L, D, 6 * D])
    bada_d = din("b_ada", [L, 1, 6 * D])
    n1_d = din("norm1T", [L, 128, 8])
    n2_d = din("norm2T", [L, 128, 8])
    win_d = din("w_in", [L, D, NIN])
    sink_d = din("sinkb", [L, 128, 8])
    an_d = din("anormb", [L, 128, 512])
    lbl_d = din("lblT", [128, L, 8])
    hgn_d = din("hgnT", [128, L])
    wout_d = din("w_out", [L, D, D])
    wr_d = din("w_router", [L, D, E])
    br_d = din("brb", [L, 128, E])
    wgu_d = din("w_gu", [L, E, D, 2 * D])
    bgu_d = din("bguT", [L, 128, E, 16])
    wd_d = din("w_down", [L, E, D, D])
    bd_d = din("b_down", [L, E, D])
    fn_d = din("fnb", [128, D])
    cd = {n: din("c_" + n, sh, dt) for n, sh, dt in CONST_SPECS}
    y_d = nc.dram_tensor("y", [S, D], F32, kind="ExternalOutput").ap()

    st = contextlib.ExitStack()
    with st:
        NW = 53200
        arena_t = st.enter_context(nc.sbuf_tensor("arena", [128, NW], F32))
        A = Arena(arena_t, NW)
        banks = []
        for i in range(8):
            t = st.enter_context(nc.psum_tensor("pb%d" % i, [128, 512], F32))
            banks.append((t, Buf(excl=True)))
        bctr = [0]

        def nb():
            b = banks[bctr[0] % 8]
            bctr[0] += 1
            return b

        P = Prog(nc)
        A.prog = P

        def mm(out, lhsT, rhs, R, W, start=True, stop=True, skip=False):
            P.op(PE, lambda e: e.matmul(out, lhsT=lhsT, rhs=rhs, start=start, stop=stop, skip_group_check=skip), R, W)

        def tr(out, in_, ident, R, W):
            P.op(PE, lambda e: e.transpose(out, in_, ident), R, W)

        def act(out, in_, func, R, W, scale=1.0, bias=None, accum=None):
            def f(e):
                kw = {}
                if bias is not None:
                    kw["bias"] = bias
                if accum is not None:
                    kw["accum_out"] = accum
                return e.activation(out=out, in_=in_, func=func, scale=scale, **kw)
            P.op(ACT, f, R, W)

        def ts(out, in0, s1, s2, op0, op1, R, W):
            if s2 is None:
                P.op(DVE, lambda e: e.tensor_scalar(out=out, in0=in0, scalar1=s1, scalar2=None, op0=op0), R, W)
            else:
                P.op(DVE, lambda e: e.tensor_scalar(out=out, in0=in0, scalar1=s1, scalar2=s2, op0=op0, op1=op1), R, W)

        def tt(out, in0, in1, op, R, W):
            P.op(DVE, lambda e: e.tensor_tensor(out=out, in0=in0, in1=in1, op=op), R, W)

        def stt(out, in0, sc, in1, op0, op1, R, W, accum=None):
            if accum is None:
                P.op(DVE, lambda e: e.scalar_tensor_tensor(out=out, in0=in0, scalar=sc, in1=in1, op0=op0, op1=op1), R, W)
            else:
                P.op(DVE, lambda e: e.scalar_tensor_tensor(out=out, in0=in0, scalar=sc, in1=in1, op0=op0, op1=op1, accum_out=accum), R, W)

        def cp(out, in_, R, W):
            P.op(DVE, lambda e: e.tensor_copy(out=out, in_=in_), R, W)

        def recip(out, in_, R, W):
            P.op(DVE, lambda e: e.reciprocal(out=out, in_=in_), R, W)

        def dma(eng, out, in_, slot, R, W, ncok=False):
            if ncok:
                P.dma(eng, lambda e: e.dma_start(out=out, in_=in_, allow_slow_non_contiguous=True), slot, R, W)
            else:
                P.dma(eng, lambda e: e.dma_start(out=out, in_=in_), slot, R, W)

        def rsqrt_chain(dst, src, scale, R_, W_, tmpb):
            ts(dst, src, scale, EPS, ALU.mult, ALU.add, R_, W_)
            act(dst, dst, AF.Sqrt, W_, W_)
            recip(dst, dst, W_, W_)

        X, _ = A.alloc([NT, D])
        xb = [Buf() for _ in range(NT)]
        K = {}
        for n, sh, dt in CONST_SPECS:
            K[n], kb_ = A.alloc(sh[1:], dt)
            K[n + "_b"] = kb_
        for n, sh, dt in CONST_SPECS:
            np_ = sh[0]
            dma(SP, K[n][0:np_], cd[n], "const", [], [K[n + "_b"]])
        ident_bf, ident_f, ones_bf, ones_f = K["ident_bf"], K["ident_f"], K["ones_bf"], K["ones_f"]
        CB = [K[n + "_b"] for n, _, _ in CONST_SPECS]
        condT, condb = A.alloc([8])
        lbT, lbb = A.alloc([L, 8])
        omlT, _ = A.alloc([L, 8])
        lbm1T, _ = A.alloc([L, 8])
        hgnT, hgnb = A.alloc([L])
        gbc, gbcb = A.alloc([2, D])
        modT, modTb = A.alloc([4, 8])
        hT, _ = A.alloc([8, S], BF16)
        hb = [Buf() for _ in range(NQ)]
        ropeC, ropeb = A.alloc([S])
        ropeS, _ = A.alloc([S])

        dma(SP, X, x_d.rearrange("(i p) d -> p i d", p=128), "x", [], xb)
        dma(SP, condT, cT_d, "small", [], [condb])
        dma(SP, hgnT, hgn_d, "small", [], [hgnb])

        m0 = A.mark()
        t8, t8b = A.alloc([8])
        act(t8, condT, AF.Sigmoid, [condb], [t8b])
        tt(condT, condT, t8, ALU.mult, [condb, t8b], [condb])
        lraw, lrawb = A.alloc([L, 8])
        lsum, lsumb = A.alloc([8])
        dma(SP, lraw, lbl_d, "small", [], [lrawb])
        act(lraw, lraw, AF.Exp, [lrawb], [lrawb])
        cp(lsum, lraw[:, 0, :], [lrawb], [lsumb])
        for l in range(1, L):
            tt(lsum, lsum, lraw[:, l, :], ALU.add, [lsumb, lrawb], [lsumb])
        recip(lsum, lsum, [lsumb], [lsumb])
        P.op(DVE, lambda e: e.memset(lbT[:, 0, :], 0.0), [], [lbb])
        for l in range(1, L):
            tt(t8, lraw[:, l, :], lsum, ALU.mult, [lrawb, lsumb], [t8b])
            tt(lbT[:, l, :], lbT[:, l - 1, :], t8, ALU.add, [lbb, t8b], [lbb])
        ts(omlT, lbT, -1.0, 1.0, ALU.mult, ALU.add, [lbb], [lbb])
        ts(lbm1T, lbT, -1.0, None, ALU.add, None, [lbb], [lbb])
        TWO_PI = float(2 * np.pi)
        posi, posb = A.alloc([S], I32)
        ang, angb = A.alloc([S])
        rr, rrb = A.alloc([S])
        ni, nib = A.alloc([S], I32)
        dma(SP, posi, pos_d, "small", [], [posb])
        rc = K["ropec"]
        cp(ang, posi, [posb], [angb])
        ts(ang, ang, rc[:, 0:1], None, ALU.mult, None, [angb, K["ropec_b"]], [angb])
        ts(rr, ang, 1.0 / TWO_PI, None, ALU.mult, None, [angb], [rrb])
        cp(ni, rr, [rrb], [nib])
        cp(rr, ni, [nib], [rrb])
        stt(ang, rr, -TWO_PI, ang, ALU.mult, ALU.add, [rrb, angb], [angb])

        def wrap_sin(dst, shift, dstb):
            ts(rr, ang, shift, None, ALU.add, None, [angb], [rrb])
            ts(ni.bitcast(F32), rr, float(np.pi), -TWO_PI, ALU.is_gt, ALU.mult, [rrb], [nib])
            tt(rr, rr, ni.bitcast(F32), ALU.add, [rrb, nib], [rrb])
            ts(ni.bitcast(F32), rr, float(-np.pi), TWO_PI, ALU.is_lt, ALU.mult, [rrb], [nib])
            tt(rr, rr, ni.bitcast(F32), ALU.add, [rrb, nib], [rrb])
            act(dst, rr, AF.Sin, [rrb], [dstb])

        wrap_sin(ropeC, float(np.pi / 2), ropeb)
        wrap_sin(ropeS, 0.0, ropeb)
        ts(ropeS, ropeS, rc[:, 1:2], None, ALU.mult, None, [ropeb, K["ropec_b"]], [ropeb])
        A.release(m0)

        def norm_phase(l, which, router=None):
            m = A.mark()
            ss, ssb = A.alloc([NT])
            junk, junkb = A.alloc([D])
            xn_dt = F32 if router is not None else BF16
            xn = [A.alloc([D], xn_dt) for _ in range(4)]
            for i in range(NT):
                act(junk, X[:, i, :], AF.Square, [xb[i]], [junkb, ssb], accum=ss[:, i:i + 1])
            rsqrt_chain(ss, ss, 1.0 / D, [ssb], [ssb], None)
            aT = modT[:, 2 * which, :]
            bT = modT[:, 2 * which + 1, :]
            if router is not None:
                hn, hnb = A.alloc([8, 512])
            for q in range(NQ):
                for ii in range(4):
                    i = q * 4 + ii
                    ts(xn[ii][0], X[:, i, :], ss[:, i:i + 1], None, ALU.mult, None, [xb[i], ssb], [xn[ii][1]])
                for k in range(8):
                    bk, bkb = nb()
                    if router is None:
                        bv = bk.bitcast(BF16)
                        for ii in range(4):
                            tr(bv[:, ii * 128:(ii + 1) * 128], xn[ii][0][:, k * 128:(k + 1) * 128], ident_bf, [xn[ii][1]] + CB, [bkb])
                        ts(hT[:, k, q * 512:(q + 1) * 512], bv[:, 0:512], aT[:, k:k + 1], bT[:, k:k + 1], ALU.mult, ALU.add,
                           [bkb, modTb], [hb[q]])
                    else:
                        for ii in range(4):
                            tr(bk[:, ii * 128:(ii + 1) * 128], xn[ii][0][:, k * 128:(k + 1) * 128], ident_f, [xn[ii][1]] + CB, [bkb])
                        ts(hn[:, k, :], bk[:, 0:512], aT[:, k:k + 1], bT[:, k:k + 1], ALU.mult, ALU.add, [bkb, modTb], [hnb])
                        act(hT[:, k, q * 512:(q + 1) * 512], hn[:, k, :], AF.Copy, [hnb], [hb[q]])
                if router is not None:
                    router(q, hn, hnb)
            A.release(m)

        def x_update(i, half, psum, psb, gidx, gate=None, gate_b=None):
            tmp, tmpb = xtmp[0]
            xtc[0] += 1
            sl = slice(half * 512, (half + 1) * 512)
            tt(tmp, psum, gbc[:, gidx, sl], ALU.mult, [psb, gbcb], [tmpb])
            if gate is None:
                tt(X[:, i, sl], X[:, i, sl], tmp, ALU.add, [xb[i], tmpb], [xb[i]])
            else:
                stt(X[:, i, sl], tmp, gate, X[:, i, sl], ALU.mult, ALU.add, [tmpb, gate_b, xb[i]], [xb[i]])

        def x_add(i, half, psum, psb):
            sl = slice(half * 512, (half + 1) * 512)
            tt(X[:, i, sl], X[:, i, sl], psum, ALU.add, [xb[i], psb], [xb[i]])

        xtmp = [A.alloc([512]) for _ in range(1)]
        xtc = [0]
        win_v = [win_d[l].rearrange("(k p) n -> p k n", p=128) for l in range(L)]
        wout_v = [wout_d[l].rearrange("(k p) n -> p k n", p=128) for l in range(L)]

        for l in range(L if stop != "pro" else 0):
            m = A.mark()
            modrow, modrowb = A.alloc([6 * D])
            badar = [A.alloc([512]) for _ in range(2)]
            wblk = [A.alloc([8, 512]) for _ in range(2)]
            n12, n12b = A.alloc([2, 8])
            dma(SP, n12[:, 0, :], n1_d[l], "small", [], [n12b])
            dma(SP, n12[:, 1, :], n2_d[l], "small", [], [n12b])
            wav = wada_d[l].rearrange("(k p) n -> p k n", p=128)
            for j in range(12):
                wb_, wbb = wblk[j % 2]
                dma(SP, wb_, wav[:, :, j * 512:(j + 1) * 512], ("wada", j % 2), [], [wbb])
                bd_, bdb_ = badar[j % 2]
                dma(SP, bd_[0:1], bada_d[l][:, j * 512:(j + 1) * 512], ("bada", j % 2), [], [bdb_])
                bk, bkb = nb()
                for k in range(8):
                    mm(bk[0:1, :], condT[:, k:k + 1], wb_[:, k, :], [condb, wbb], [bkb], start=(k == 0), stop=(k == 7))
                tt(modrow[0:1, j * 512:(j + 1) * 512], bk[0:1, :], bd_[0:1, :], ALU.add,
                   [bkb, bdb_], [modrowb])
            for gi, v in enumerate((2, 5)):
                for hf in range(2):
                    bk, bkb = nb()
                    mm(bk[:, :], ones_f[0:1, 0:128], modrow[0:1, v * D + hf * 512: v * D + (hf + 1) * 512], [modrowb] + CB, [bkb])
                    cp(gbc[:, gi, hf * 512:(hf + 1) * 512], bk[:, :], [bkb], [gbcb])
            bk, bkb = nb()
            for vi, v in enumerate((1, 0, 4, 3)):
                for k in range(8):
                    col = (vi * 8 + k) * 2
                    mm(bk[:, col:col + 2], modrow[0:1, v * D + k * 128: v * D + (k + 1) * 128], ones_f[0:1, 0:2], [modrowb] + CB, [bkb])
            bkv = bk[:, 0:64].rearrange("p (v k two) -> p v k two", v=4, k=8)
            cp(modT, bkv[:, :, :, 0], [bkb], [modTb])
            for w_ in range(2):
                ts(modT[:, 2 * w_, :], modT[:, 2 * w_, :], 1.0, None, ALU.add, None, [modTb], [modTb])
                tt(modT[:, 2 * w_, :], modT[:, 2 * w_, :], n12[:, w_, :], ALU.mult, [modTb, n12b], [modTb])
            A.release(m)

            if stop == "ada":
                continue
            norm_phase(l, 0)
            if stop == "n1":
                continue

            for hh in range(4):
                m = A.mark()
                Wh, Whb = A.alloc([8, 5, 128], BF16)
                woh, wohb = A.alloc([D], BF16)
                for j, c0 in enumerate((768, 1280, 1792, 2304, 2816)):
                    dma(POOL, Wh[:, :, j, :], win_v[l][:, :, c0 + hh * 128: c0 + (hh + 1) * 128], ("wh", j), [], [Whb])
                dma(POOL, woh, wout_d[l][512 + hh * 128: 512 + (hh + 1) * 128, :], ("wh", 5), [], [wohb])
                tt(woh, woh, gbc[:, 0, :], ALU.mult, [wohb, gbcb], [wohb])
                qside = [A.alloc([S], BF16) for _ in range(2)]
                kside = [A.alloc([S], BF16) for _ in range(2)]
                ktok = [A.alloc([NT, 128], BF16) for _ in range(2)]
                vtok, vtokb = A.alloc([NT, 128], BF16)
                Gt = [A.alloc([NCH]) for _ in range(2)]
                oacc, oaccb = A.alloc([S])
                T = [A.alloc([512]) for _ in range(8)]
                for q in range(NQ):
                    qs = slice(q * 512, (q + 1) * 512)
                    bk, bkb = nb()
                    for k in range(8):
                        mm(bk[:, :], Wh[:, k, 0, :], hT[:, k, qs], [Whb, hb[q]], [bkb], start=(k == 0), stop=(k == 7))
                    act(T[0][0], bk[:, :], AF.Sigmoid, [bkb], [T[0][1]])
                    tt(T[1][0], bk[:, :], T[0][0], ALU.mult, [bkb, T[0][1]], [T[1][1]])
                    for d in range(2):
                        li = d * 4 + hh
                        bk, bkb = nb()
                        for k in range(8):
                            mm(bk[:, :], Wh[:, k, 1 + d, :], hT[:, k, qs], [Whb, hb[q]], [bkb], start=(k == 0), stop=(k == 7))
                        act(T[2][0], bk[:, :], AF.Sigmoid, [bkb], [T[2][1]])
                        ts(T[3][0], T[2][0], omlT[:, l, li:li + 1], lbT[:, l, li:li + 1], ALU.mult, ALU.add, [T[2][1], lbb], [T[3][1]])
                        ts(T[4][0], T[2][0], lbm1T[:, l, li:li + 1], omlT[:, l, li:li + 1], ALU.mult, ALU.add, [T[2][1], lbb], [T[4][1]])
                        act(T[3][0], T[3][0], AF.Ln, [T[3][1]], [T[3][1]])
                        P.op(DVE, lambda e, o=T[5][0], a=K["rmask"], b_=T[3][0]: e.tensor_tensor_scan(
                            out=o, data0=a, data1=b_, initial=0.0, op0=ALU.mult, op1=ALU.add), [T[3][1], K["rmask_b"]], [T[5][1]])
                        bend = T[5][0].rearrange("p (c j) -> p c j", j=32)[:, :, 31]
                        if d == 0:
                            act(T[6][0], T[5][0], AF.Exp, [T[5][1]], [T[6][1]])
                            act(T[7][0], T[5][0], AF.Exp, [T[5][1]], [T[7][1]], scale=-1.0)
                        else:
                            tt(T[3][0], T[5][0], T[3][0], ALU.subtract, [T[5][1], T[3][1]], [T[3][1]])
                            act(T[6][0], T[3][0], AF.Exp, [T[3][1]], [T[6][1]], scale=-1.0)
                            act(T[7][0], T[3][0], AF.Exp, [T[3][1]], [T[7][1]])
                        act(Gt[d][0][:, q * 16:(q + 1) * 16], bend, AF.Exp, [T[5][1]], [Gt[d][1]])
                        tt(qside[d][0][:, qs], T[1][0], T[6][0], ALU.mult, [T[1][1], T[6][1]], [qside[d][1]])
                        tt(kside[d][0][:, qs], T[4][0], T[7][0], ALU.mult, [T[4][1], T[7][1]], [kside[d][1]])
                        bk, bkb = nb()
                        bv = bk.bitcast(BF16)
                        for ii in range(4):
                            tr(bv[:, ii * 128:(ii + 1) * 128], kside[d][0][:, q * 512 + ii * 128: q * 512 + (ii + 1) * 128], ident_bf,
                               [kside[d][1]] + CB, [bkb])
                        cp(ktok[d][0][:, q * 4:(q + 1) * 4, :], bv[:, 0:512].rearrange("p (i c) -> p i c", i=4), [bkb], [ktok[d][1]])
                    bk, bkb = nb()
                    for ii in range(4):
                        tsl = slice(q * 512 + ii * 128, q * 512 + (ii + 1) * 128)
                        for k in range(8):
                            mm(bk[:, ii * 128:(ii + 1) * 128], hT[:, k, tsl], Wh[:, k, 3, :], [Whb, hb[q]], [bkb], start=(k == 0), stop=(k == 7))
                    cp(vtok[:, q * 4:(q + 1) * 4, :], bk[:, :].rearrange("p (i c) -> p i c", i=4), [bkb], [vtokb])
                Wst = [A.alloc([128]) for _ in range(4)]
                Ub = [A.alloc([16, 128], BF16) for _ in range(1)]
                Vm = [A.alloc([4, 4, 128], BF16) for _ in range(1)]
                Abd = [A.alloc([4, 128], BF16) for _ in range(1)]
                onT = [A.alloc([512], BF16) for _ in range(1)]
                wcnt = 0
                have_prev = False
                for d in range(2):
                    have_prev = False
                    qorder = range(NQ) if d == 0 else range(NQ - 1, -1, -1)
                    for qi_, q in enumerate(qorder):
                        qs = slice(q * 512, (q + 1) * 512)
                        vm, vmb = Vm[0]
                        ub, ubb = Ub[0]
                        abd, abdb = Abd[0]
                        for ii in range(4):
                            for r in range(4):
                                ts(vm[:, ii, r, :], vtok[:, q * 4 + ii, :], K["bm4"][:, r:r + 1], None, ALU.mult, None,
                                   [vtokb, K["bm4_b"]], [vmb])
                        zb = []
                        for ii in range(4):
                            bk, bkb = nb()
                            mm(bk[:, :], ktok[d][0][:, q * 4 + ii, :], vm[:, ii, :, :], [ktok[d][1], vmb], [bkb])
                            zb.append((bk, bkb))
                        corder = range(16) if d == 0 else range(15, -1, -1)
                        uvalid = [False] * 16
                        for cl in corder:
                            c = q * 16 + cl
                            zc = zb[cl // 4][0][:, (cl % 4) * 128:(cl % 4 + 1) * 128]
                            zcb = zb[cl // 4][1]
                            wnew, wnewb = Wst[wcnt % 4]
                            wold, woldb = Wst[(wcnt - 1) % 4]
                            if not have_prev:
                                cp(wnew, zc, [zcb], [wnewb])
                            else:
                                gidx = (c - 1) if d == 0 else c
                                gsc = Gt[d][0][:, gidx:gidx + 1]
                                act(ub[:, cl, :], wold, AF.Copy, [woldb, Gt[d][1]], [ubb], scale=gsc)
                                uvalid[cl] = True
                                stt(wnew, wold, gsc, zc, ALU.mult, ALU.add, [woldb, Gt[d][1], zcb], [wnewb])
                            have_prev = True
                            wcnt += 1
                        bk, bkb = nb()
                        for ii in range(4):
                            tsl = slice(q * 512 + ii * 128, q * 512 + (ii + 1) * 128)
                            mm(bk[:, ii * 128:(ii + 1) * 128], kside[d][0][:, tsl], qside[d][0][:, tsl], [kside[d][1], qside[d][1]], [bkb])
                        tt(abd, bk[:, :].rearrange("p (i c) -> p i c", i=4), K["hmask"][:, d, :, :], ALU.mult, [bkb, K["hmask_b"]], [abdb])
                        ob, obb = nb()
                        first = True
                        for ii in range(4):
                            mm(ob[:, ii * 128:(ii + 1) * 128], vtok[:, q * 4 + ii, :], abd[:, ii, :], [vtokb, abdb], [obb],
                               start=first, stop=False, skip=True)
                            first = False
                        nl = sum(uvalid)
                        cnt = 0
                        for cl in range(16):
                            if not uvalid[cl]:
                                continue
                            cnt += 1
                            mm(ob[:, cl * 32:(cl + 1) * 32], ub[:, cl, :], qside[d][0][:, q * 512 + cl * 32: q * 512 + (cl + 1) * 32],
                               [ubb, qside[d][1]], [obb], start=False, stop=(cnt == nl), skip=True)
                        if d == 0:
                            cp(oacc[:, qs], ob[:, :], [obb], [oaccb])
                        else:
                            o_, o_b = T[0]
                            tt(o_, ob[:, :], oacc[:, qs], ALU.add, [obb, oaccb], [o_b])
                            act(T[1][0].bitcast(BF16)[:, 0:512], o_, AF.Square, [o_b], [T[1][1]])
                            bk, bkb = nb()
                            mm(bk[:, :], ones_bf, T[1][0].bitcast(BF16)[:, 0:512], [T[1][1]] + CB, [bkb])
                            ts(T[2][0], bk[:, :], 1.0 / 128, EPS, ALU.mult, ALU.add, [bkb], [T[2][1]])
                            act(T[2][0], T[2][0], AF.Sqrt, [T[2][1]], [T[2][1]])
                            recip(T[2][0], T[2][0], [T[2][1]], [T[2][1]])
                            stt(T[3][0], o_, hgnT[:, l:l + 1], T[2][0], ALU.mult, ALU.mult, [o_b, hgnb, T[2][1]], [T[3][1]])
                            bk, bkb = nb()
                            for k in range(8):
                                mm(bk[:, :], Wh[:, k, 4, :], hT[:, k, qs], [Whb, hb[q]], [bkb], start=(k == 0), stop=(k == 7))
                            act(T[4][0], bk[:, :], AF.Sigmoid, [bkb], [T[4][1]])
                            tt(T[4][0], bk[:, :], T[4][0], ALU.mult, [bkb, T[4][1]], [T[4][1]])
                            on_, onb_ = onT[0]
                            tt(on_, T[3][0], T[4][0], ALU.mult, [T[3][1], T[4][1]], [onb_])
                            for ii in range(4):
                                for hf in range(2):
                                    bk, bkb = nb()
                                    mm(bk[:, :], on_[:, ii * 128:(ii + 1) * 128], woh[:, hf * 512:(hf + 1) * 512], [onb_, wohb], [bkb])
                                    x_add(q * 4 + ii, hf, bk[:, :], bkb)
                A.release(m)

            if stop == "hg":
                continue
            m = A.mark()
            lim = int(stop[2:]) if (stop or "").startswith("at") else 99
            Wq2, Wqb = A.alloc([8, 512], BF16)
            Wqs2, Wqsb = A.alloc([8, 512], BF16)
            woa, woab = A.alloc([4, D], BF16)
            kz = [[A.alloc([S], BF16) for _ in range(2)] for _ in range(2)]
            Va, Vab = A.alloc([NT, 2, 66], BF16)
            esink, esinkb = A.alloc([8])
            anb, anbb = A.alloc([512])
            r1 = [A.alloc([128]) for _ in range(2)]
            r2 = [A.alloc([128]) for _ in range(2)]
            mk = A.mark()
            Wk2, Wkb = A.alloc([8, 128], BF16)
            Wkd, Wkdb = A.alloc([8, 2, 128], BF16)
            Wksd, Wksdb = A.alloc([8, 2, 128], BF16)
            Wv, Wvb = A.alloc([8, 128], BF16)
            dma(POOL, Wq2, win_v[l][:, :, 0:512], ("wa", 0), [], [Wqb])
            dma(POOL, Wk2, win_v[l][:, :, 512:640], ("wa", 1), [], [Wkb])
            dma(POOL, Wv, win_v[l][:, :, 640:768], ("wa", 2), [], [Wvb])
            dma(POOL, woa, wout_v[l][:, 0:4, :], ("wa", 3), [], [woab])
            for c_ in range(4):
                tt(woa[:, c_, :], woa[:, c_, :], gbc[:, 0, :], ALU.mult, [woab, gbcb], [woab])
            cp(Wqs2, Wq2, [Wqb], [Wqsb])
            q4 = Wq2.rearrange("p k (h e) -> p k h e", h=8)
            qs4 = Wqs2.rearrange("p k (h e) -> p k h e", h=8)
            cp(qs4[:, :, :, 0:8], q4[:, :, :, 8:16], [Wqb], [Wqsb])
            cp(qs4[:, :, :, 8:16], q4[:, :, :, 0:8], [Wqb], [Wqsb])
            for g in range(2):
                for hf in range(2):
                    cp(Wkd[:, :, g, hf * 64:(hf + 1) * 64], Wk2[:, :, g * 64:(g + 1) * 64], [Wkb], [Wkdb])
                    cp(Wksd[:, :, g, hf * 64:(hf + 1) * 64], Wk2[:, :, g * 64:(g + 1) * 64], [Wkb], [Wksdb])
                    cp(Wksd[:, :, g, hf * 64:hf * 64 + 8], Wk2[:, :, g * 64 + 8:g * 64 + 16], [Wkb], [Wksdb])
                    cp(Wksd[:, :, g, hf * 64 + 8:hf * 64 + 16], Wk2[:, :, g * 64:g * 64 + 8], [Wkb], [Wksdb])
            dma(SP, esink, sink_d[l], "small", [], [esinkb])
            dma(SP, anb, an_d[l], "small", [], [anbb])
            act(esink, esink, AF.Exp, [esinkb], [esinkb])
            P.op(DVE, lambda e: e.memset(Va[:, :, :, 64:66], 1.0), [], [Vab])
            for g in range(2):
                for par in range(2):
                    P.op(DVE, lambda e, t_=kz[g][par][0]: e.memset(t_, 0.0), [], [kz[g][par][1]])
            rcn = 0
            for g in range(2 if lim >= 2 else 0):
                for q in range(NQ):
                    qs = slice(q * 512, (q + 1) * 512)
                    bk, bkb = nb()
                    for k in range(8):
                        mm(bk[:, :], Wkd[:, k, g, :], hT[:, k, qs], [Wkdb, hb[q]], [bkb], start=(k == 0), stop=(k == 7))
                    b2, b2b = nb()
                    for k in range(8):
                        mm(b2[:, :], Wksd[:, k, g, :], hT[:, k, qs], [Wksdb, hb[q]], [b2b], start=(k == 0), stop=(k == 7))
                    for par in range(2):
                        pr = slice(par * 64, par * 64 + 64)
                        rr_ = slice(par * 64, par * 64 + 16)
                        kzt, kzb = kz[g][par]
                        act(kzt[pr, qs], bk[pr, :], AF.Copy, [bkb], [kzb])
                        a1, a1b = r1[rcn % 2]
                        a2, a2b = r2[rcn % 2]
                        rcn += 1
                        for ii in range(4):
                            sl = slice(q * 512 + ii * 128, q * 512 + (ii + 1) * 128)
                            if ii > 0:
                                a1, a1b = r1[rcn % 2]
                                a2, a2b = r2[rcn % 2]
                                rcn += 1
                            tt(a1[rr_], bk[rr_, ii * 128:(ii + 1) * 128], ropeC[rr_, sl], ALU.mult, [bkb, ropeb], [a1b])
                            tt(a2[rr_], b2[rr_, ii * 128:(ii + 1) * 128], ropeS[rr_, sl], ALU.mult, [b2b, ropeb], [a2b])
                            tt(kzt[rr_, sl], a1[rr_], a2[rr_], ALU.add, [a1b, a2b], [kzb])
            for q in range(NQ if lim >= 3 else 0):
                bk, bkb = nb()
                for ii in range(4):
                    tsl = slice(q * 512 + ii * 128, q * 512 + (ii + 1) * 128)
                    for k in range(8):
                        mm(bk[:, ii * 128:(ii + 1) * 128], hT[:, k, tsl], Wv[:, k, :], [Wvb, hb[q]], [bkb], start=(k == 0), stop=(k == 7))
                cp(Va[:, q * 4:(q + 1) * 4, :, 0:64], bk[:, :].rearrange("p (i g e) -> p i g e", i=4, g=2), [bkb], [Vab])
            A.release(mk)
            qT = [A.alloc([4, 128], BF16) for _ in range(2)]
            PT = [A.alloc([4, 128], BF16) for _ in range(6)]
            of32 = [A.alloc([512]) for _ in range(2)]
            onb2 = [A.alloc([512], BF16) for _ in range(2)]
            aTt = [A.alloc([4, 128], BF16) for _ in range(2)]
            sm = [A.alloc([24]) for _ in range(2)]
            junk2, junk2b = A.alloc([512])
            ptc = 0
            for n in range(NT if lim >= 4 else 0):
                q = n // 4
                nsl = slice(n * 128, (n + 1) * 128)
                qt, qtb = qT[n % 2]
                bk, bkb = nb()
                b2, b2b = nb()
                for c4 in range(4):
                    for k in range(8):
                        mm(bk[:, c4 * 128:(c4 + 1) * 128], Wq2[:, k, c4 * 128:(c4 + 1) * 128], hT[:, k, nsl], [Wqb, hb[q]], [bkb], start=(k == 0), stop=(k == 7))
                    for k in range(8):
                        mm(b2[:, c4 * 128:(c4 + 1) * 128], Wqs2[:, k, c4 * 128:(c4 + 1) * 128], hT[:, k, nsl], [Wqsb, hb[q]], [b2b], start=(k == 0), stop=(k == 7))
                act(qt, bk[:, :].rearrange("p (c t) -> p c t", c=4), AF.Copy, [bkb], [qtb])
                for c4 in range(4):
                    for par in range(2):
                        rr_ = slice(par * 64, par * 64 + 16)
                        a1, a1b = r1[rcn % 2]
                        a2, a2b = r2[rcn % 2]
                        rcn += 1
                        tt(a1[rr_], bk[rr_, c4 * 128:(c4 + 1) * 128], ropeC[rr_, nsl], ALU.mult, [bkb, ropeb], [a1b])
                        tt(a2[rr_], b2[rr_, c4 * 128:(c4 + 1) * 128], ropeS[rr_, nsl], ALU.mult, [b2b, ropeb], [a2b])
                        tt(qt[rr_, c4, :], a1[rr_], a2[rr_], ALU.add, [a1b, a2b], [qtb])
                of_, ofb = of32[n % 2]
                sm_, smb = sm[n % 2]
                if lim < 5:
                    continue
                for g in range(2):
                    js = [j for j in (n - 1, n, n + 1) if 0 <= j < NT]
                    pts = []
                    for j in js:
                        sb_, sbb = nb()
                        for par in range(2):
                            mm(sb_[:, par * 256:(par + 1) * 256], kz[g][par][0][:, j * 128:(j + 1) * 128], qt[:, 2 * g:2 * g + 2, :],
                               [kz[g][par][1], qtb], [sbb])
                        pt, ptb = PT[ptc % 6]
                        ptc += 1
                        act(pt, sb_[:, :].rearrange("p (h c) -> p h c", h=4), AF.Exp, [sbb], [ptb], scale=0.125)
                        if j != n:
                            mi = 0 if j == n - 1 else 1
                            tt(pt, pt, K["amask"][:, mi, :, :], ALU.mult, [ptb, K["amask_b"]], [ptb])
                        pts.append((pt, ptb))
                    if lim < 6:
                        continue
                    ob, obb = nb()
                    for s4 in range(4):
                        for ji, j in enumerate(js):
                            mm(ob[:, s4 * 66:(s4 + 1) * 66], pts[ji][0][:, s4, :], Va[:, j, g, :], [pts[ji][1], Vab], [obb],
                               start=(ji == 0), stop=(ji == len(js) - 1), skip=True)
                    obv = ob[:, 0:264].rearrange("p (h e) -> p h e", h=4)
                    tt(sm_[:, g * 4:(g + 1) * 4].rearrange("p (par cc) -> p par cc", par=2),
                       obv[:, :, 64].rearrange("p (par cc) -> p par cc", par=2),
                       esink[:, g * 4:(g + 1) * 4].rearrange("p (cc par) -> p par cc", par=2), ALU.add, [obb, esinkb], [smb])
                    recip(sm_[:, 8 + g * 4: 8 + (g + 1) * 4], sm_[:, g * 4:(g + 1) * 4], [smb], [smb])
                    for s4 in range(4):
                        h = 4 * g + 2 * (s4 % 2) + (s4 // 2)
                        ts(of_[:, h * 64:(h + 1) * 64], obv[:, s4, 0:64], sm_[:, 8 + g * 4 + s4: 9 + g * 4 + s4], None, ALU.mult, None, [obb, smb], [ofb])
                if lim < 7:
                    continue
                act(junk2, of_, AF.Square, [ofb], [junk2b, smb], accum=sm_[:, 16:17])
                rsqrt_chain(sm_[:, 16:17], sm_[:, 16:17], 1.0 / 512, [smb], [smb], None)
                on_, onb_ = onb2[n % 2]
                stt(on_, of_, sm_[:, 16:17], anb, ALU.mult, ALU.mult, [ofb, smb, anbb], [onb_])
                bk, bkb = nb()
                bv = bk.bitcast(BF16)
                for c4 in range(4):
                    tr(bv[:, c4 * 128:(c4 + 1) * 128], on_[:, c4 * 128:(c4 + 1) * 128], ident_bf, [onb_] + CB, [bkb])
                at_, atb = aTt[n % 2]
                cp(at_, bv[:, 0:512].rearrange("p (c t) -> p c t", c=4), [bkb], [atb])
                for hf in range(2):
                    bk, bkb = nb()
                    for c4 in range(4):
                        mm(bk[:, :], at_[:, c4, :], woa[:, c4, hf * 512:(hf + 1) * 512], [atb, woab], [bkb], start=(c4 == 0), stop=(c4 == 3))
                    x_add(n, hf, bk[:, :], bkb)
            A.release(m)
            if stop == "mix":
                continue

            m = A.mark()
            G, Gb = A.alloc([NT, E])
            wr, wrb = A.alloc([8, E])
            brb, brbb = A.alloc([E])
            bgu, bgub = A.alloc([E, 16])
            m2 = A.mark()
            GT, GTb = A.alloc([S])
            bdn, bdnb = A.alloc([D])
            dma(SP, wr, wr_d[l].rearrange("(k p) e -> p k e", p=128), "small", [], [wrb], ncok=True)
            dma(SP, brb, br_d[l], "small", [], [brbb])
            dma(SP, bgu, bgu_d[l], "small", [], [bgub])
            P.op(DVE, lambda e: e.memset(bdn, 0.0), [], [bdnb])
            dma(SP, bdn[0:E], bd_d[l], "small", [], [bdnb])
            ts(bgu[:, :, 8:16], bgu[:, :, 8:16], 1.0, None, ALU.add, None, [bgub], [bgub])
            Gp, Gpb = A.alloc([128])
            P.op(DVE, lambda e: e.memset(Gp, 0.0), [], [Gpb])
            rt = [A.alloc([3, E]) for _ in range(2)]
            rs_ = [A.alloc([16]) for _ in range(2)]

            def router(q, hn, hnb):
                for ii in range(4):
                    i = q * 4 + ii
                    lg3, lgb = rt[i % 2]
                    s_, s_b = rs_[i % 2]
                    bk, bkb = nb()
                    for k in range(8):
                        mm(bk[:, 0:E], hn[:, k, ii * 128:(ii + 1) * 128], wr[:, k, :], [hnb, wrb], [bkb], start=(k == 0), stop=(k == 7))
                    tt(lg3[:, 0, :], bk[:, 0:E], brb, ALU.add, [bkb, brbb], [lgb])
                    P.op(DVE, lambda e, o=s_[:, 0:8], a=lg3[:, 0, :]: e.max(out=o, in_=a), [lgb], [s_b])
                    ts(s_[:, 8:9], s_[:, 0:1], -1.0, None, ALU.mult, None, [s_b], [s_b])
                    act(lg3[:, 1, :], lg3[:, 0, :], AF.Exp, [lgb, s_b], [lgb], bias=s_[:, 8:9])
                    stt(lg3[:, 2, :], lg3[:, 0, :], s_[:, 3:4], lg3[:, 1, :], ALU.is_ge, ALU.mult, [lgb, s_b], [lgb, s_b], accum=s_[:, 9:10])
                    recip(s_[:, 10:11], s_[:, 9:10], [s_b], [s_b])
                    ts(G[:, i, :], lg3[:, 2, :], s_[:, 10:11], None, ALU.mult, None, [lgb, s_b], [Gb])
                    cp(Gp[:, 0:E], G[:, i, :], [Gb], [Gpb])
                    bk, bkb = nb()
                    tr(bk[:, 0:128], Gp, ident_f, [Gpb] + CB, [bkb])
                    cp(GT[:, i * 128:(i + 1) * 128], bk[:, 0:128], [bkb], [GTb])

            norm_phase(l, 1, router=router)
            for i in range(NT):
                for hf in range(2):
                    bk, bkb = nb()
                    mm(bk[:, :], GT[:, i * 128:(i + 1) * 128], bdn[:, hf * 512:(hf + 1) * 512], [GTb, bdnb], [bkb])
                    x_update(i, hf, bk[:, :], bkb, 1)

            A.release(m2)
            Wg = [A.alloc([8, 2, 512], BF16) for _ in range(2)]
            Wd = [A.alloc([4, D], BF16) for _ in range(2)]
            actT = [A.alloc([4, 512], BF16) for _ in range(2)]
            Tm = [[A.alloc([512]), A.alloc([512], BF16), A.alloc([512]), A.alloc([512], BF16)] for _ in range(2)]
            fcn = 0
            ac = 0
            units = [(e_, f2) for e_ in range(E) for f2 in range(2)]

            def load_unit(ui):
                e_, f2 = units[ui]
                wg, wgb = Wg[ui % 2]
                wd, wdb = Wd[ui % 2]
                wgv = wgu_d[l, e_].rearrange("(k p) (j n) -> p k j n", p=128, j=2)
                for j_ in range(2):
                    dma(POOL, wg[:, :, j_, :], wgv[:, :, j_, f2 * 512:(f2 + 1) * 512], ("wg", ui % 2), [], [wgb])
                dma(POOL, wd, wd_d[l, e_, f2 * 512:(f2 + 1) * 512, :].rearrange("(c p) n -> p c n", p=128), ("wd", ui % 2), [], [wdb])

            load_unit(0)
            for ui in range(len(units)):
                e_, f2 = units[ui]
                if ui + 1 < len(units):
                    load_unit(ui + 1)
                wg, wgb = Wg[ui % 2]
                wd, wdb = Wd[ui % 2]
                for c_ in range(4):
                    tt(wd[:, c_, :], wd[:, c_, :], gbc[:, 1, :], ALU.mult, [wdb, gbcb], [wdb])
                def gu_part(q, at_, atb):
                    nonlocal fcn
                    qs = slice(q * 512, (q + 1) * 512)
                    for fc4 in range(4):
                        fc = f2 * 4 + fc4
                        tm = Tm[fcn % 2]
                        fcn += 1
                        pg, pgb = nb()
                        for k in range(8):
                            mm(pg[:, :], wg[:, k, 0, fc4 * 128:(fc4 + 1) * 128], hT[:, k, qs], [wgb, hb[q]], [pgb], start=(k == 0), stop=(k == 7))
                        pl, plb = nb()
                        for k in range(8):
                            mm(pl[:, :], wg[:, k, 1, fc4 * 128:(fc4 + 1) * 128], hT[:, k, qs], [wgb, hb[q]], [plb], start=(k == 0), stop=(k == 7))
                        ts(tm[0][0], pg[:, :], bgu[:, e_, fc:fc + 1], 7.0, ALU.add, ALU.min, [pgb, bgub], [tm[0][1]])
                        act(tm[1][0], tm[0][0], AF.Sigmoid, [tm[0][1]], [tm[1][1]], scale=1.702)
                        ts(tm[2][0], pl[:, :], bgu[:, e_, 8 + fc:9 + fc], 8.0, ALU.add, ALU.min, [plb, bgub], [tm[2][1]])
                        P.op(POOL, lambda e, o=tm[3][0], a=tm[0][0], b_=tm[1][0]: e.tensor_tensor(out=o, in0=a, in1=b_, op=ALU.mult),
                             [tm[0][1], tm[1][1]], [tm[3][1]])
                        stt(at_[:, fc4, :], tm[2][0], -6.0, tm[3][0], ALU.max, ALU.mult, [tm[2][1], tm[3][1]], [atb])

                def down_part(q, at_, atb):
                    for ii in range(4):
                        i = q * 4 + ii
                        for hf in range(2):
                            pd, pdb = nb()
                            for fc4 in range(4):
                                mm(pd[:, :], at_[:, fc4, ii * 128:(ii + 1) * 128], wd[:, fc4, hf * 512:(hf + 1) * 512], [atb, wdb], [pdb],
                                   start=(fc4 == 0), stop=(fc4 == 3))
                            sl_ = slice(hf * 512, (hf + 1) * 512)
                            stt(X[:, i, sl_], pd[:, :], G[:, i, e_:e_ + 1], X[:, i, sl_], ALU.mult, ALU.add, [pdb, Gb, xb[i]], [xb[i]])

                prev = None
                for q in range(NQ):
                    cur = actT[ac % 2]
                    ac += 1
                    gu_part(q, cur[0], cur[1])
                    if prev is not None:
                        down_part(prev[0], prev[1][0], prev[1][1])
                    prev = (q, cur)
                down_part(prev[0], prev[1][0], prev[1][1])
            A.release(m)

        m = A.mark()
        ss, ssb = A.alloc([NT])
        junk, junkb = A.alloc([D])
        yo = [A.alloc([D]) for _ in range(2)]
        fnb, fnbb = A.alloc([D])
        dma(SP, fnb, fn_d, "small", [], [fnbb])
        for i in range(NT):
            act(junk, X[:, i, :], AF.Square, [xb[i]], [junkb, ssb], accum=ss[:, i:i + 1])
        rsqrt_chain(ss, ss, 1.0 / D, [ssb], [ssb], None)
        yv = y_d.rearrange("(i p) d -> i p d", p=128)
        finals = []
        for i in range(NT):
            yo_, yob = yo[i % 2]
            stt(yo_, X[:, i, :], ss[:, i:i + 1], fnb, ALU.mult, ALU.mult, [xb[i], ssb, fnbb], [yob])
            finals.append(P.dma(SP, lambda e, o=yv[i], a=yo_: e.dma_start(out=o, in_=a), ("y", i % 2), [yob], []))
        A.release(m)
        P.emit(final_waits=finals)
        build.info = dict(n_ins=len(P.ins), n_sems=P.n_sems, sbuf_peak_words=A.peak)
    return nc


def make_in_maps(inp, S, E, L):
    B = inp["x"].shape[0]
    f = lambda a: np.ascontiguousarray(np.asarray(a))
    cst = _consts()
    shared = {
        "w_ada": f(inp["w_ada"]), "b_ada": f(np.asarray(inp["b_ada"]).reshape(L, 1, 6 * D)),
        "norm1T": f(np.asarray(inp["norm1"]).reshape(L, 8, 128).transpose(0, 2, 1)),
        "norm2T": f(np.asarray(inp["norm2"]).reshape(L, 8, 128).transpose(0, 2, 1)),
        "w_in": f(inp["w_in"]),
        "sinkb": f(np.broadcast_to(np.asarray(inp["attn_sink"])[:, None, :], (L, 128, 8))),
        "anormb": f(np.broadcast_to(np.asarray(inp["attn_norm"])[:, None, :], (L, 128, 512))),
        "lblT": f(np.asarray(inp["hg_lb_logits"]).reshape(L, 2, 4, 128).transpose(3, 0, 1, 2).reshape(128, L, 8)),
        "hgnT": f(np.asarray(inp["hg_norm"]).T),
        "w_out": f(inp["w_out"]), "w_router": f(inp["w_router"]),
        "brb": f(np.broadcast_to(np.asarray(inp["b_router"])[:, None, :], (L, 128, E))),
        "w_gu": f(inp["w_gu"]),
        "bguT": f(np.asarray(inp["b_gu"]).reshape(L, E, 16, 128).transpose(0, 3, 1, 2)),
        "w_down": f(inp["w_down"]), "b_down": f(inp["b_down"]),
        "fnb": f(np.broadcast_to(np.asarray(inp["final_norm"])[None, :], (128, D))),
    }
    for n, _, _ in CONST_SPECS:
        shared["c_" + n] = cst[n]
    maps = []
    for b in range(B):
        m = dict(shared)
        m["x"] = f(inp["x"][b])
        m["cT"] = f(np.asarray(inp["c"][b]).reshape(8, 128).T)
        m["pos16"] = f(np.broadcast_to(np.asarray(inp["positions"][b]).astype(np.int32)[None, :], (128, S)))
        maps.append(m)
    return maps


def kernel(**inputs):
    B, S, _ = inputs["x"].shape
    L = inputs["w_ada"].shape[0]
    E = inputs["w_router"].shape[2]
    nc = build(S, E, L)
    maps = make_in_maps(inputs, S, E, L)
    res = run_bass_kernel_spmd(nc, maps, core_ids=list(range(B)))
    return np.stack([np.asarray(r["y"]) for r in res.results], axis=0).astype(np.float32)
```
